# Optimizing a Trainium2 kernel written in Bass

```python
import math
import jax, jax.numpy as jnp
from jax import lax
import numpy as np

D_MODEL = 1024
BATCH = 4
SEQ = 4096
DEPTH = 1

CHUNK = 64
EPS = 1e-6
NEG_INF = -1e30

BRANCH_WIDTH = D_MODEL // 2
N_BRANCH = 3

A_HEADS = 8
A_HEAD_DIM = BRANCH_WIDTH // A_HEADS
A_LEFT_CHUNKS = 8
A_BAND = A_LEFT_CHUNKS + 1
REL_CLIP = 128

B_HEADS = 4
B_HEAD_DIM = BRANCH_WIDTH // B_HEADS
CONV_WIDTH = 4

MEM_TOKENS = 256
M_HEADS = 4
M_HEAD_DIM = BRANCH_WIDTH // M_HEADS

PEER_HEADS = 8
PEER_NKEYS = 128
PEER_EXPERTS = PEER_NKEYS * PEER_NKEYS
PEER_KEY_DIM = 256
PEER_HALF = PEER_KEY_DIM // 2
PEER_TOPK = 16
PEER_BLOCK = 128

IN_SPLITS = (
    BRANCH_WIDTH, BRANCH_WIDTH, BRANCH_WIDTH,
    BRANCH_WIDTH, BRANCH_WIDTH, BRANCH_WIDTH,
    BRANCH_WIDTH,
    B_HEADS, B_HEADS,
    BRANCH_WIDTH,
    N_BRANCH * D_MODEL,
)
IN_COLS = sum(IN_SPLITS)

kernel_name = "hybrid_chunkattn_gdn_memxattn_peer"


def rmsnorm(x, g):
    xf = x.astype(jnp.float32)
    y = xf * lax.rsqrt(jnp.mean(xf * xf, axis=-1, keepdims=True) + EPS)
    return (y * g.astype(jnp.float32)).astype(x.dtype)


def l2norm(x):
    xf = x.astype(jnp.float32)
    return xf * lax.rsqrt(jnp.sum(xf * xf, axis=-1, keepdims=True) + EPS)


def chunked_rel_attention(q, k, v, rel_bias):
    B_, S_, H, hd = q.shape
    nc = S_ // CHUNK
    qc = q.reshape(B_, nc, CHUNK, H, hd)
    kc = k.reshape(B_, nc, CHUNK, H, hd)
    vc = v.reshape(B_, nc, CHUNK, H, hd)
    pad = ((0, 0), (A_LEFT_CHUNKS, 0), (0, 0), (0, 0), (0, 0))
    kp = jnp.pad(kc, pad)
    vp = jnp.pad(vc, pad)
    kband = jnp.concatenate([kp[:, j:j + nc] for j in range(A_BAND)], axis=2)
    vband = jnp.concatenate([vp[:, j:j + nc] for j in range(A_BAND)], axis=2)
    s = jnp.einsum('bcqhd,bckhd->bhcqk', qc, kband).astype(jnp.float32) * (hd ** -0.5)
    qpos = jnp.arange(CHUNK)
    kpos = jnp.arange(A_BAND * CHUNK) - A_LEFT_CHUNKS * CHUNK
    rel = qpos[:, None] - kpos[None, :]
    idx = jnp.clip(rel, -REL_CLIP, REL_CLIP) + REL_CLIP
    bias = rel_bias[:, idx].astype(jnp.float32)
    kchunk = jnp.arange(nc)[:, None] - A_LEFT_CHUNKS + (jnp.arange(A_BAND * CHUNK) // CHUNK)[None, :]
    valid = kchunk >= 0
    s = s + bias[None, :, None, :, :]
    s = jnp.where(valid[None, None, :, None, :], s, NEG_INF)
    p = jax.nn.softmax(s, axis=-1).astype(v.dtype)
    o = jnp.einsum('bhcqk,bckhd->bcqhd', p, vband)
    return o.reshape(B_, S_, H * hd)


def causal_conv_silu(x, w):
    y = lax.conv_general_dilated(
        x, w.astype(x.dtype), window_strides=(1,), padding=[(CONV_WIDTH - 1, 0)],
        dimension_numbers=('NWC', 'WIO', 'NWC'), feature_group_count=x.shape[-1])
    return jax.nn.silu(y)


def gated_delta_rule(q, k, v, g, beta):
    f32 = jnp.float32
    B_, S_, H, dk = q.shape
    dv = v.shape[-1]
    nc = S_ // CHUNK

    def to_chunks(t):
        return t.astype(f32).reshape(B_, nc, CHUNK, H, -1).transpose(0, 3, 1, 2, 4)

    qc, kc, vc = to_chunks(q), to_chunks(k), to_chunks(v)
    gc = jnp.cumsum(g.astype(f32).reshape(B_, nc, CHUNK, H).transpose(0, 3, 1, 2), axis=-1)
    bc = beta.astype(f32).reshape(B_, nc, CHUNK, H).transpose(0, 3, 1, 2)
    tri_incl = jnp.tril(jnp.ones((CHUNK, CHUNK), dtype=bool))
    tri_strict = jnp.tril(jnp.ones((CHUNK, CHUNK), dtype=bool), k=-1)
    gamma = jnp.exp(jnp.where(tri_incl, gc[..., :, None] - gc[..., None, :], -jnp.inf))
    kk = jnp.einsum('bhnid,bhnjd->bhnij', kc, kc)
    a_strict = jnp.where(tri_strict, bc[..., :, None] * kk * gamma, 0.0)
    eye = jnp.eye(CHUNK, dtype=f32)
    rhs = jnp.concatenate([vc * bc[..., None], kc * (bc * jnp.exp(gc))[..., None]], axis=-1)
    sol = lax.linalg.triangular_solve(eye + a_strict, rhs, left_side=True, lower=True,
                                      unit_diagonal=True)
    u, w = sol[..., :dv], sol[..., dv:]
    qk = jnp.where(tri_incl, jnp.einsum('bhnid,bhnjd->bhnij', qc, kc) * gamma, 0.0)
    q_dec = qc * jnp.exp(gc)[..., None]
    k_dec = kc * jnp.exp(gc[..., -1:] - gc)[..., None]
    g_last = jnp.exp(gc[..., -1])
    xs = (jnp.moveaxis(u, 2, 0), jnp.moveaxis(w, 2, 0), jnp.moveaxis(qk, 2, 0),
          jnp.moveaxis(q_dec, 2, 0), jnp.moveaxis(k_dec, 2, 0), jnp.moveaxis(g_last, 2, 0))

    def step(state, inp):
        u_c, w_c, qk_c, qd_c, kd_c, gl_c = inp
        v_new = u_c - jnp.einsum('bhcd,bhde->bhce', w_c, state)
        o_c = jnp.einsum('bhcd,bhde->bhce', qd_c, state) + jnp.einsum('bhij,bhje->bhie', qk_c, v_new)
        state = state * gl_c[..., None, None] + jnp.einsum('bhcd,bhce->bhde', kd_c, v_new)
        return state, o_c

    s0 = jnp.zeros((B_, H, dk, dv), f32)
    _, o = lax.scan(step, s0, xs)
    return o.transpose(1, 0, 3, 2, 4).reshape(B_, S_, H, dv)


def memory_cross_attention(qm, mem, g_mem, w_mem_kv, gq, gk):
    B_, S_, _ = qm.shape
    M_ = mem.shape[1]
    kv = rmsnorm(mem, g_mem) @ w_mem_kv
    km, vm = jnp.split(kv, 2, axis=-1)
    q = rmsnorm(qm.reshape(B_, S_, M_HEADS, M_HEAD_DIM), gq)
    k = rmsnorm(km.reshape(B_, M_, M_HEADS, M_HEAD_DIM), gk)
    v = vm.reshape(B_, M_, M_HEADS, M_HEAD_DIM)
    s = jnp.einsum('bshd,bmhd->bhsm', q, k).astype(jnp.float32) * (M_HEAD_DIM ** -0.5)
    p = jax.nn.softmax(s, axis=-1).astype(v.dtype)
    return jnp.einsum('bhsm,bmhd->bshd', p, v).reshape(B_, S_, M_HEADS * M_HEAD_DIM)


def peer(xn, w_q, sub_keys, expert_u, expert_v):
    B_, S_, D = xn.shape
    T = B_ * S_
    xt = xn.reshape(T, D)
    qry = (xt @ w_q).reshape(T, PEER_HEADS, 2, PEER_HALF)
    sc = jnp.einsum('thpc,pnc->thpn', qry, sub_keys).astype(jnp.float32)
    s1, i1 = lax.top_k(sc[:, :, 0], PEER_TOPK)
    s2, i2 = lax.top_k(sc[:, :, 1], PEER_TOPK)
    cand_s = (s1[..., :, None] + s2[..., None, :]).reshape(T, PEER_HEADS, PEER_TOPK * PEER_TOPK)
    cand_i = (i1[..., :, None] * PEER_NKEYS + i2[..., None, :]).reshape(T, PEER_HEADS, PEER_TOPK * PEER_TOPK)
    top_s, pos = lax.top_k(cand_s, PEER_TOPK)
    eidx = jnp.take_along_axis(cand_i, pos, axis=-1)
    gate = jax.nn.softmax(top_s, axis=-1).astype(xn.dtype)
    nb = T // PEER_BLOCK

    def block(args):
        xb, eb, gb = args
        u = expert_u[eb]
        v = expert_v[eb]
        a = jnp.einsum('thkd,td->thk', u, xb)
        return jnp.einsum('thk,thkd->td', gb * jax.nn.gelu(a, approximate=False), v)

    y = lax.map(block, (xt.reshape(nb, PEER_BLOCK, D),
                        eidx.reshape(nb, PEER_BLOCK, PEER_HEADS, PEER_TOPK),
                        gate.reshape(nb, PEER_BLOCK, PEER_HEADS, PEER_TOPK)))
    return y.reshape(B_, S_, D)


def setup_inputs(seed: int = 0) -> dict:
    key = jax.random.key(seed)
    ks = jax.random.split(key, 24)
    f32 = jnp.float32
    L, D = DEPTH, D_MODEL

    def nrm(k, shape, scale):
        return jax.random.normal(k, shape, f32) * scale

    def gain(k, n):
        return 1.0 + 0.05 * jax.random.normal(k, (L, n), f32)

    dt = jnp.exp(jax.random.uniform(ks[8], (L, B_HEADS), f32, math.log(1e-3), math.log(1e-1)))
    dt_bias = dt + jnp.log(-jnp.expm1(-dt))
    return {
        "x": nrm(ks[0], (BATCH, SEQ, D), 1.0),
        "mem": nrm(ks[1], (BATCH, MEM_TOKENS, D), 1.0),
        "g_mix_norm": gain(ks[2], D),
        "w_in": nrm(ks[3], (L, D, IN_COLS), D ** -0.5),
        "a_q_norm": gain(ks[4], A_HEAD_DIM),
        "a_k_norm": gain(ks[5], A_HEAD_DIM),
        "a_rel_bias": nrm(ks[6], (L, A_HEADS, 2 * REL_CLIP + 1), 0.1),
        "b_conv": nrm(ks[7], (L, CONV_WIDTH, 1, 3 * BRANCH_WIDTH), CONV_WIDTH ** -0.5),
        "b_a_log": jnp.log(jax.random.uniform(ks[9], (L, B_HEADS), f32, 1.0, 16.0)),
        "b_dt_bias": dt_bias,
        "b_out_norm": gain(ks[10], B_HEAD_DIM),
        "g_mem_norm": gain(ks[11], D),
        "w_mem_kv": nrm(ks[12], (L, D, 2 * BRANCH_WIDTH), D ** -0.5),
        "m_q_norm": gain(ks[13], M_HEAD_DIM),
        "m_k_norm": gain(ks[14], M_HEAD_DIM),
        "w_branch": nrm(ks[15], (L, N_BRANCH, BRANCH_WIDTH, D), BRANCH_WIDTH ** -0.5),
        "w_out": nrm(ks[16], (L, D, D), D ** -0.5),
        "g_ffn_norm": gain(ks[17], D),
        "w_peer_q": nrm(ks[18], (L, D, PEER_HEADS * PEER_KEY_DIM), D ** -0.5),
        "peer_sub_keys": nrm(ks[19], (L, 2, PEER_NKEYS, PEER_HALF), PEER_HALF ** -0.5),
        "peer_u": nrm(ks[20], (L, PEER_EXPERTS, D), D ** -0.5),
        "peer_v": nrm(ks[21], (L, PEER_EXPERTS, D), PEER_HEADS ** -0.5),
    }


def reference(x, mem, g_mix_norm, w_in, a_q_norm, a_k_norm, a_rel_bias, b_conv, b_a_log,
              b_dt_bias, b_out_norm, g_mem_norm, w_mem_kv, m_q_norm, m_k_norm, w_branch,
              w_out, g_ffn_norm, w_peer_q, peer_sub_keys, peer_u, peer_v):
    f32 = jnp.float32
    B_, S_, _ = x.shape
    offsets = [int(o) for o in np.cumsum(IN_SPLITS)[:-1]]
    h = x
    for l in range(DEPTH):
        n = rmsnorm(h, g_mix_norm[l])
        proj = n @ w_in[l]
        (qa, ka, va, qb, kb, vb, zb, beta_raw, alpha_raw, qm, gate_raw) = jnp.split(proj, offsets, axis=-1)

        qa = rmsnorm(qa.reshape(B_, S_, A_HEADS, A_HEAD_DIM), a_q_norm[l])
        ka = rmsnorm(ka.reshape(B_, S_, A_HEADS, A_HEAD_DIM), a_k_norm[l])
        va = va.reshape(B_, S_, A_HEADS, A_HEAD_DIM)
        o_a = chunked_rel_attention(qa, ka, va, a_rel_bias[l])

        qkv = causal_conv_silu(jnp.concatenate([qb, kb, vb], axis=-1), b_conv[l])
        qb, kb, vb = jnp.split(qkv, 3, axis=-1)
        qb = l2norm(qb.reshape(B_, S_, B_HEADS, B_HEAD_DIM)) * (B_HEAD_DIM ** -0.5)
        kb = l2norm(kb.reshape(B_, S_, B_HEADS, B_HEAD_DIM))
        vb = vb.reshape(B_, S_, B_HEADS, B_HEAD_DIM)
        beta = jax.nn.sigmoid(beta_raw.astype(f32))
        g = -jnp.exp(b_a_log[l].astype(f32)) * jax.nn.softplus(alpha_raw.astype(f32) + b_dt_bias[l].astype(f32))
        o_b = gated_delta_rule(qb, kb, vb, g, beta).astype(x.dtype)
        o_b = rmsnorm(o_b, b_out_norm[l]) * jax.nn.silu(zb.reshape(B_, S_, B_HEADS, B_HEAD_DIM))
        o_b = o_b.reshape(B_, S_, BRANCH_WIDTH)

        o_m = memory_cross_attention(qm, mem, g_mem_norm[l], w_mem_kv[l], m_q_norm[l], m_k_norm[l])

        branches = jnp.stack([o_a, o_b, o_m], axis=2)
        y = jnp.einsum('bsnw,nwd->bsnd', branches, w_branch[l])
        gates = jax.nn.sigmoid(gate_raw.reshape(B_, S_, N_BRANCH, D_MODEL))
        h = h + jnp.einsum('bsnd,bsnd->bsd', gates, y) @ w_out[l]

        h = h + peer(rmsnorm(h, g_ffn_norm[l]), w_peer_q[l], peer_sub_keys[l], peer_u[l], peer_v[l])
    return h
```

```python
from contextlib import ExitStack
import numpy as np
import ml_dtypes
import concourse.bass as bass
import concourse.mybir as mybir
from concourse.bass_utils import run_bass_kernel_spmd

F32 = mybir.dt.float32
BF16 = mybir.dt.bfloat16
U32 = mybir.dt.uint32
I32 = mybir.dt.int32
AF = mybir.ActivationFunctionType
ALU = mybir.AluOpType
AX = mybir.AxisListType

N_DMA_SEMS = 24
ENGS = ("sp", "act", "pool", "pe", "dve")
EPS = 1e-6
NEG = -30000.0

T = 2048
D = 1024
INC = 7176


class Op:
    __slots__ = ("eng", "fn", "deps", "dma", "needed", "sig", "idx", "presem")

    def __init__(self, eng, fn, dma):
        self.eng = eng
        self.fn = fn
        self.dma = dma
        self.deps = []
        self.needed = False
        self.sig = None
        self.presem = None


def _nm(x):
    sub = None
    if isinstance(x, tuple):
        x, sub = x
    t = getattr(x, "tensor", None)
    name = t.name if t is not None else x.name
    return name, sub


class Prog:
    def __init__(self, nc):
        self.nc = nc
        self.ops = []
        self.st = {}
        self.last_on = {}
        self.dma_since = []

    def _deps_r(self, name, sub, deps):
        s = self.st.get(name)
        if s is None:
            return
        w = s["w"]
        if sub is None:
            deps.update(w.values())
        else:
            if sub in w:
                deps.add(w[sub])
            if None in w:
                deps.add(w[None])

    def _deps_w(self, name, sub, deps):
        self._deps_r(name, sub, deps)
        s = self.st.get(name)
        if s is None:
            return
        r = s["r"]
        if sub is None:
            for l in r.values():
                deps.update(l)
        else:
            deps.update(r.get(sub, ()))
            deps.update(r.get(None, ()))

    def op(self, eng, fn, ins=(), outs=(), dma=False):
        o = Op(eng, fn, dma)
        o.idx = len(self.ops)
        deps = set()
        rk = [_nm(x) for x in ins]
        wk = [_nm(x) for x in outs]
        for n, s in rk:
            self._deps_r(n, s, deps)
        for n, s in wk:
            self._deps_w(n, s, deps)
        o.deps = sorted(deps)
        for n, s in rk:
            st = self.st.setdefault(n, {"w": {}, "r": {}})
            st["r"].setdefault(s, []).append(o.idx)
        for n, s in wk:
            st = self.st.setdefault(n, {"w": {}, "r": {}})
            if s is None:
                st["w"] = {None: o.idx}
                st["r"] = {}
            else:
                st["w"][s] = o.idx
                st["r"][s] = []
        self.ops.append(o)
        self.last_on[eng] = o.idx
        if dma:
            self.dma_since.append(o.idx)
        return o

    def barrier(self):
        deps = sorted(set(list(self.last_on.values()) + self.dma_since))
        self.dma_since = []
        new = []
        for e in ENGS:
            o = Op(e, lambda eng: eng.nop(), False)
            o.idx = len(self.ops)
            o.deps = [d for d in deps]
            self.ops.append(o)
            new.append(o.idx)
        for e, i in zip(ENGS, new):
            self.last_on[e] = i
        self.st = {}

    def dma(self, out, in_, ins=None, outs=None, eng="sp"):
        return self.op(eng, lambda e: e.dma_start(out=out, in_=in_),
                       [in_] if ins is None else ins, [out] if outs is None else outs, dma=True)

    def mm(self, out, lhsT, rhs, start=True, stop=True, ins=None, outs=None):
        i = [lhsT, rhs] if ins is None else ins
        return self.op("pe", lambda e: e.matmul(out, lhsT=lhsT, rhs=rhs, start=start, stop=stop),
                       i, [out] if outs is None else outs)

    def tr(self, out, in_, ident, ins=None, outs=None):
        return self.op("pe", lambda e: e.transpose(out=out, in_=in_, identity=ident),
                       [in_, ident] if ins is None else ins, [out] if outs is None else outs)

    def act(self, out, in_, func, bias=None, scale=None, accum_out=None, ins=None, outs=None):
        kw = {}
        i = [in_]
        if bias is not None:
            kw["bias"] = bias
            if not isinstance(bias, (int, float)):
                i.append(bias)
        if scale is not None:
            kw["scale"] = scale
            if not isinstance(scale, (int, float)):
                i.append(scale)
        o = [out]
        if accum_out is not None:
            kw["accum_out"] = accum_out
            o.append(accum_out)
        return self.op("act", lambda e: e.activation(out=out, in_=in_, func=func, **kw),
                       i if ins is None else ins, o if outs is None else outs)

    def tt(self, out, in0, in1, op, eng="dve", ins=None, outs=None):
        return self.op(eng, lambda e: e.tensor_tensor(out=out, in0=in0, in1=in1, op=op),
                       [in0, in1] if ins is None else ins, [out] if outs is None else outs)

    def ts(self, out, in0, s1, op0, s2=None, op1=None, eng="dve", accum_out=None, ins=None, outs=None):
        i = [in0]
        for s in (s1, s2):
            if s is not None and not isinstance(s, (int, float)):
                i.append(s)
        kw = {}
        if op1 is not None:
            kw["op1"] = op1
        o = [out]
        if accum_out is not None:
            kw["accum_out"] = accum_out
            o.append(accum_out)
        return self.op(eng, lambda e: e.tensor_scalar(out=out, in0=in0, scalar1=s1, scalar2=s2, op0=op0, **kw),
                       i if ins is None else ins, o if outs is None else outs)

    def stt(self, out, in0, scalar, in1, op0, op1, accum_out=None, ins=None, outs=None):
        i = [in0, in1]
        if not isinstance(scalar, (int, float)):
            i.append(scalar)
        kw = {}
        o = [out]
        if accum_out is not None:
            kw["accum_out"] = accum_out
            o.append(accum_out)
        return self.op("dve", lambda e: e.scalar_tensor_tensor(out=out, in0=in0, scalar=scalar, in1=in1,
                                                               op0=op0, op1=op1, **kw),
                       i if ins is None else ins, o if outs is None else outs)

    def copy(self, out, in_, eng="dve", ins=None, outs=None):
        if eng == "act":
            f = lambda e: e.copy(out=out, in_=in_)
        else:
            f = lambda e: e.tensor_copy(out=out, in_=in_)
        return self.op(eng, f, [in_] if ins is None else ins, [out] if outs is None else outs)

    def recip(self, out, in_):
        return self.op("dve", lambda e: e.reciprocal(out=out, in_=in_), [in_], [out])

    def memset(self, out, val, eng="pool"):
        return self.op(eng, lambda e: e.memset(out, val), [], [out])

    def reduce(self, out, in_, op=ALU.add, axis=AX.X):
        return self.op("dve", lambda e: e.tensor_reduce(out=out, in_=in_, axis=axis, op=op), [in_], [out])

    def emit(self, sems, dma_sems):
        ops = self.ops
        for o in ops:
            latest = {}
            keep = []
            for d in o.deps:
                od = ops[d]
                if od.dma:
                    keep.append(d)
                else:
                    if od.eng == "pe" and o.eng == "pe" and not o.dma:
                        continue
                    if od.eng not in latest or latest[od.eng] < d:
                        latest[od.eng] = d
            keep.extend(latest.values())
            o.deps = sorted(keep)
            for d in o.deps:
                ops[d].needed = True
        cnt = {e: 0 for e in ENGS}
        nds = len(dma_sems)
        dma_use = [0] * nds
        rr = 0
        for o in ops:
            if o.dma:
                s = rr % nds
                rr += 1
                if dma_use[s] > 0:
                    o.presem = (dma_sems[s], 16 * dma_use[s])
                dma_use[s] += 1
                o.sig = (dma_sems[s], 16 * dma_use[s])
            elif o.needed:
                cnt[o.eng] += 1
                o.sig = (sems[o.eng], cnt[o.eng])
        per_eng = {e: [o for o in ops if o.eng == e] for e in ENGS}
        final_dma = [(dma_sems[s], 16 * dma_use[s]) for s in range(nds) if dma_use[s] > 0]
        nc = self.nc

        def run(eng_name, e):
            waited = {}

            def wait(sem, val):
                k = id(sem)
                if waited.get(k, 0) >= val:
                    return
                waited[k] = val
                e.wait_ge(sem, val)

            for o in per_eng[eng_name]:
                for d in o.deps:
                    sem, val = ops[d].sig
                    wait(sem, val)
                if o.presem is not None:
                    wait(*o.presem)
                ins = o.fn(e)
                if o.sig is not None:
                    sem, val = o.sig
                    ins.then_inc(sem, 16 if o.dma else 1)
            if eng_name == "sp":
                for sem, val in final_dma:
                    wait(sem, val)

        with nc.Block() as block:
            @block.sync
            def _(e):
                run("sp", e)

            @block.scalar
            def _(e):
                run("act", e)

            @block.gpsimd
            def _(e):
                run("pool", e)

            @block.tensor
            def _(e):
                run("pe", e)

            @block.vector
            def _(e):
                run("dve", e)


class Ctx:
    pass


def build_nc(dbg=()):
    nc = bass.Bass("TRN2", target_bir_lowering=False)
    P = Prog(nc)
    C = Ctx()
    C.nc, C.P = nc, P

    def din(name, shape, dt=F32):
        return nc.dram_tensor(name, list(shape), dt, kind="ExternalInput").ap()

    def dscr(name, shape, dt=F32):
        kind = "ExternalOutput" if name in dbg else "Internal"
        return nc.dram_tensor(name, list(shape), dt, kind=kind).ap()

    I = C.I = {}
    I["xo"] = din("xo", [T, D])
    I["xp"] = din("xp", [T, D])
    I["mem"] = din("mem", [256, D])
    I["g_mix"] = din("g_mix", [1, D])
    I["w_in"] = din("w_in", [D, INC])
    I["aqn"] = din("aqn", [128, 1])
    I["akn"] = din("akn", [128, 1])
    I["abias"] = din("abias", [64, 8, 576])
    I["hmask"] = din("hmask", [64, 8, 576])
    I["convw"] = din("convw", [128, 12, 4])
    I["b_a_log"] = din("b_a_log", [1, 4])
    I["b_dt_bias"] = din("b_dt_bias", [1, 4])
    I["b_out_norm"] = din("b_out_norm", [1, 128])
    I["g_mem"] = din("g_mem", [1, D])
    I["w_mem_kv"] = din("w_mem_kv", [D, D])
    I["mqn"] = din("mqn", [128, 1])
    I["mkn"] = din("mkn", [128, 1])
    I["w_branch"] = din("w_branch", [3, 512, D])
    I["w_out"] = din("w_out", [D, D])
    I["g_ffn"] = din("g_ffn", [1, D])
    I["w_peer_q"] = din("w_peer_q", [D, 2048])
    I["skT"] = din("skT", [128, 2, 128])
    I["utr"] = din("utr", [128, 128, 1024])
    I["peer_v"] = din("peer_v", [16384, D])
    out = C.out = nc.dram_tensor("out", [T, D], F32, kind="ExternalOutput").ap()

    S = C.S = {}
    S["qaT"] = dscr("qaT", [4, 128, T], BF16)
    S["kaT"] = dscr("kaT", [4, 128, T + 512], BF16)
    S["va"] = dscr("va", [T + 512, 512], BF16)
    S["qmT"] = dscr("qmT", [4, 128, T], BF16)
    S["obT"] = dscr("obT", [4, 128, T], BF16)
    S["nT"] = dscr("nT", [8, 128, T], BF16)
    S["oaT"] = dscr("oaT", [64, 8, T], BF16)
    S["omT"] = dscr("omT", [4, 128, T], BF16)
    S["h1"] = dscr("h1", [T, D], F32)
    S["xn2T"] = dscr("xn2T", [8, 128, T], BF16)
    S["i1T"] = dscr("i1T", [128, T], F32)
    S["i2T"] = dscr("i2T", [128, T], F32)
    S["gT"] = dscr("gT", [128, T], F32)
    S["ub"] = dscr("ub", [128, 128, 1024], BF16)
    S["vb"] = dscr("vb", [16384, D], BF16)

    with ExitStack() as top:
        sems = {e: top.enter_context(nc.semaphore("s_" + e)) for e in ENGS}
        dsems = [top.enter_context(nc.semaphore(f"dq{i}")) for i in range(N_DMA_SEMS)]

        def gsb(name, shape, dt):
            return top.enter_context(nc.sbuf_tensor(name, list(shape), dt))

        K = C.K = {}
        identf = K["identf"] = gsb("identf", [128, 128], F32)
        ident = K["ident"] = gsb("ident", [128, 128], BF16)
        onesf = K["onesf"] = gsb("onesf", [128, 128], F32)
        negonesf = K["negonesf"] = gsb("negonesf", [128, 128], F32)
        onesb = K["onesb"] = gsb("onesb", [128, 128], BF16)
        bd64 = K["bd64"] = gsb("bd64", [128, 128], F32)
        P.memset(onesf[:], 1.0)
        P.memset(negonesf[:], -1.0)
        P.memset(identf[:], 1.0)
        P.op("pool", lambda e: e.affine_select(out=identf[:], in_=identf[:], pattern=[[-1, 128]],
                                               compare_op=ALU.is_equal, fill=0.0, base=0,
                                               channel_multiplier=1), [identf], [identf])
        P.copy(ident[:], identf[:], eng="act")
        P.copy(onesb[:], onesf[:], eng="act")
        P.memset(bd64[:], 0.0)
        P.memset(bd64[0:64, 0:64], 1.0)
        P.memset(bd64[64:128, 64:128], 1.0)

        psb = [top.enter_context(nc.psum_tensor(f"pb{i}", [128, 512], F32)) for i in range(8)]
        C.psb = psb

        phase1(C)
        P.barrier()
        phase2(C)
        P.barrier()
        phase3(C)
        P.barrier()
        phase4(C)
        P.barrier()
        phase5(C)
        P.emit(sems, dsems)
    return nc


def _mask_tile(C, es, name, cmp_lower_incl=None, kind=None):
    nc, P = C.nc, C.P
    t = es.enter_context(nc.sbuf_tensor(name, [64, 4, 64], F32))
    if kind == "U":
        P.memset(t[:], 1.0)
        args = dict(pattern=[[0, 4], [1, 64]], compare_op=ALU.is_ge, fill=0.0, base=0, channel_multiplier=-1)
    elif kind == "SL":
        P.memset(t[:], 1.0)
        args = dict(pattern=[[0, 4], [-1, 64]], compare_op=ALU.is_gt, fill=0.0, base=0, channel_multiplier=1)
    elif kind == "ML":
        P.memset(t[:], 0.0)
        args = dict(pattern=[[0, 4], [-1, 64]], compare_op=ALU.is_ge, fill=NEG, base=0, channel_multiplier=1)
    elif kind == "MU":
        P.memset(t[:], 0.0)
        args = dict(pattern=[[0, 4], [1, 64]], compare_op=ALU.is_ge, fill=NEG, base=0, channel_multiplier=-1)
    elif kind == "S01":
        P.memset(t[:], 1.0)
        args = dict(pattern=[[0, 4], [-1, 64]], compare_op=ALU.is_gt, fill=0.0, base=0, channel_multiplier=1)
    elif kind == "I":
        P.memset(t[:], 1.0)
        args = dict(pattern=[[0, 4], [-1, 64]], compare_op=ALU.is_equal, fill=0.0, base=0, channel_multiplier=1)
    P.op("pool", lambda e: e.affine_select(out=t[:], in_=t[:], **args), [t], [t])
    return t


def phase1(C):
    nc, P, I, S, K, psb = C.nc, C.P, C.I, C.S, C.K, C.psb
    identf, ident, onesf, negonesf, onesb, bd64 = (K[k] for k in
                                                   ("identf", "ident", "onesf", "negonesf", "onesb", "bd64"))
    with ExitStack() as es:
        def sb(name, shape, dt=F32):
            return es.enter_context(nc.sbuf_tensor("p1_" + name, list(shape), dt))

        mU = _mask_tile(C, es, "p1_mU", kind="U")
        mSL = _mask_tile(C, es, "p1_mSL", kind="SL")
        mML = _mask_tile(C, es, "p1_mML", kind="ML")
        mMU = _mask_tile(C, es, "p1_mMU", kind="MU")
        mS01 = _mask_tile(C, es, "p1_mS01", kind="S01")
        mI = _mask_tile(C, es, "p1_mI", kind="I")

        Sst = sb("Sst", [128, 4, 128])
        P.memset(Sst[:], 0.0)
        Sb = sb("Sb", [128, 4, 128], BF16)
        P.memset(Sb[:], 0.0)
        hist = sb("hist", [128, 12, 3])
        P.memset(hist[:], 0.0)
        P.barrier()

        WC = 4104
        wb = sb("wb", [128, 8, WC], BF16)
        xt = [sb(f"xt{i}", [128, D]) for i in range(2)]
        stg = xt + [sb("wstg2", [128, D])]
        n = 0
        for c0 in (2048, 3072, 4096, 0, 1024):
            for kc in range(8):
                w = min(1024, WC - c0)
                st = stg[n % 3]
                P.dma(st[:, 0:w], I["w_in"][kc * 128:(kc + 1) * 128, c0:c0 + w])
                P.copy(wb[:, kc, c0:c0 + w], st[:, 0:w], eng=("act", "dve")[n % 2],
                       outs=[(wb, (kc, c0))])
                n += 1
        gt = sb("gt", [128, D])
        P.dma(gt[:], I["g_mix"].broadcast_to([128, D]))
        aqn = sb("aqn", [128, 1]); akn = sb("akn", [128, 1]); mqn = sb("mqn", [128, 1])
        P.dma(aqn[:], I["aqn"]); P.dma(akn[:], I["akn"]); P.dma(mqn[:], I["mqn"])
        P.ts(aqn[:], aqn[:], 0.125, ALU.mult)
        P.ts(mqn[:], mqn[:], float(128 ** -0.5), ALU.mult)
        convw = sb("convw", [128, 12, 4])
        P.dma(convw[:], I["convw"])
        dtb = sb("dtb", [64, 4]); negA = sb("negA", [64, 4]); gob = sb("gob", [64, 128])
        P.dma(dtb[:], I["b_dt_bias"].broadcast_to([64, 4]))
        P.dma(negA[:], I["b_a_log"].broadcast_to([64, 4]))
        P.dma(gob[:], I["b_out_norm"].broadcast_to([64, 128]))
        P.act(negA[:], negA[:], AF.Exp)
        P.ts(negA[:], negA[:], -1.0, ALU.mult)
        nbf = [sb(f"nbf{i}", [128, D], BF16) for i in range(2)]
        ss = sb("ss", [128, 1]); rstd = sb("rstd", [128, 1])
        nT = sb("nT", [128, 8, 512], BF16)
        sqr = [sb(f"sq{i}", [128, 512]) for i in range(2)]
        rsr = [sb(f"rs{i}", [128, 512]) for i in range(2)]
        obf = [sb(f"obf{i}", [128, 512], BF16) for i in range(2)]
        cbr = [sb(f"cb{i}", [128, 515]) for i in range(2)]
        cyr = [sb(f"cy{i}", [128, 512]) for i in range(2)]
        sgr = [sb(f"sg{i}", [128, 512]) for i in range(2)]
        qT = sb("qT", [128, 4, 512]); kT = sb("kT", [128, 4, 512]); vT = sb("vT", [128, 4, 512])
        qTb = sb("qTb", [128, 4, 512], BF16); kTb = sb("kTb", [128, 4, 512], BF16)
        vst = [sb(f"vst{i}", [128, 512], BF16) for i in range(2)]
        obT = sb("obT", [128, 4, 512], BF16)
        bta = sb("bta", [64, 4]); nbta = sb("nbta", [64, 4]); bwe = sb("bwe", [64, 4])
        gx = sb("gx", [64, 4]); gg = sb("gg", [64, 4])
        egc = sb("egc", [64, 4]); erev = sb("erev", [64, 4]); egl = sb("egl", [128, 4])
        GU = sb("GU", [64, 4, 64])
        EX = sb("EX", [64, 2, 4, 64])
        gamS = sb("gamS", [64, 4, 64])
        egcB = sb("egcB", [128, 4, 64])
        qdT = sb("qdT", [128, 4, 64])
        vb_ = sb("vb_", [64, 4, 128], BF16); kbe = sb("kbe", [64, 4, 128], BF16); kdec = sb("kdec", [64, 4, 128])
        Bp = sb("Bp", [64, 2, 4, 64], BF16)
        Btmp = sb("Btmp", [64, 4, 64])
        Pm = sb("Pm", [64, 4, 64], BF16)
        sgz = sb("sgz", [64, 512])
        qkT = sb("qkT", [64, 4, 64])
        u2 = [sb(f"u_{i}", [64, 4, 128]) for i in range(2)]; wT2 = [sb(f"wT{i}", [128, 4, 64], BF16) for i in range(2)]
        vn = sb("vn", [64, 4, 128], BF16)
        qkT2 = [sb(f"qkT{i}", [64, 4, 64], BF16) for i in range(2)]
        qdT2 = [sb(f"qdT{i}", [128, 4, 64], BF16) for i in range(2)]
        kdec2 = [sb(f"kdec{i}", [64, 4, 128], BF16) for i in range(2)]; egl2 = [sb(f"egl{i}", [128, 4]) for i in range(2)]
        osq = sb("osq", [64, 4, 128]); oss = sb("oss", [64, 4]); ors = sb("ors", [64, 4])
        on = sb("on", [64, 4, 128]); zs = sb("zs", [64, 512]); obc = sb("obc", [64, 512], BF16)

        def rms_stats(src_ps, npart):
            pass

        def chan_proj(co, ps):
            for kc in range(8):
                wk = [(wb, (kc, (co // 1024) * 1024))]
                if (co + 127) // 1024 != co // 1024:
                    wk.append((wb, (kc, ((co + 127) // 1024) * 1024)))
                P.mm(ps[:], lhsT=wb[:, kc, co:co + 128], rhs=nT[:, kc, :], start=(kc == 0), stop=(kc == 7),
                     ins=wk + [nT])

        evi = [0]

        def block(xsrc, t0, own, halo):
            for ti in range(4):
                x_ = xt[ti % 2]; nb = nbf[ti % 2]
                P.dma(x_[:], xsrc[t0 + ti * 128: t0 + (ti + 1) * 128, :])
                P.act(nb[:], x_[:], AF.Square, accum_out=ss[:])
                P.act(ss[:], ss[:], AF.Ln, bias=EPS, scale=1.0 / D)
                P.act(rstd[:], ss[:], AF.Exp, scale=-0.5)
                P.stt(nb[:], x_[:], rstd[:], gt[:], ALU.mult, ALU.mult)
                pT = psb[0][:].bitcast(BF16)
                for kc in range(8):
                    P.tr(pT[:, kc * 128:(kc + 1) * 128], nb[:, kc * 128:(kc + 1) * 128], ident[:],
                         outs=[psb[0]])
                P.copy(nT[:, :, ti * 128:(ti + 1) * 128],
                       pT[:, 0:1024].rearrange("p (k t) -> p k t", k=8), eng="act", ins=[psb[0]])
            tg = t0
            def norm_chunk(kind, ci):
                def gen(e_):
                    ps = psb[1 + 2 * e_]; ps2 = psb[2 + 2 * e_]; sq = sqr[e_]; rs = rsr[e_]; ob = obf[e_]
                    if kind == "qa":
                        co, gain, lhs, sc_ = ci * 128, aqn, bd64, 1.0 / 64
                    elif kind == "ka":
                        co, gain, lhs, sc_ = 512 + ci * 128, akn, bd64, 1.0 / 64
                    else:
                        co, gain, lhs, sc_ = 3592 + ci * 128, mqn, onesf, 1.0 / 128
                    chan_proj(co, ps)
                    yield
                    P.act(sq[:], ps[:], AF.Square)
                    yield
                    P.mm(ps2[:], lhsT=lhs[:], rhs=sq[:])
                    yield
                    P.act(rs[:], ps2[:], AF.Ln, bias=EPS, scale=sc_)
                    yield
                    P.act(rs[:], rs[:], AF.Exp, scale=-0.5)
                    yield
                    P.stt(ob[:], ps[:], gain[:], rs[:], ALU.mult, ALU.mult)
                    yield
                    if kind == "qa":
                        P.dma(S["qaT"][ci, :, tg:tg + 512], ob[:], outs=[(S["qaT"], (ci, tg))])
                    elif kind == "ka":
                        kt = tg + 512 if own else tg - 1536
                        P.dma(S["kaT"][ci, :, kt:kt + 512], ob[:], outs=[(S["kaT"], (ci, kt))])
                    else:
                        P.dma(S["qmT"][ci, :, tg:tg + 512], ob[:], outs=[(S["qmT"], (ci, tg))])
                    yield
                return gen

            def conv_chunk(ci):
                def gen(e_):
                    which = ci // 4
                    co = 1536 + ci * 128
                    ps = psb[1 + 2 * e_]; ps2 = psb[2 + 2 * e_]; sq = sqr[e_]; rs = rsr[e_]
                    cb = cbr[e_]; cy = cyr[e_]; sg = sgr[e_]
                    chan_proj(co, ps)
                    P.copy(cb[:, 0:3], hist[:, ci, :], eng="act")
                    yield
                    P.copy(cb[:, 3:515], ps[:], eng="dve")
                    yield
                    P.copy(hist[:, ci, :], cb[:, 512:515], eng="act")
                    P.ts(cy[:], cb[:, 0:512], convw[:, ci, 0:1], ALU.mult)
                    yield
                    for w_ in range(1, 4):
                        P.stt(cy[:], cb[:, w_:w_ + 512], convw[:, ci, w_:w_ + 1], cy[:], ALU.mult, ALU.add)
                        yield
                    P.act(sg[:], cy[:], AF.Exp, scale=-1.0)
                    yield
                    P.act(sg[:], sg[:], AF.Ln, bias=1.0)
                    yield
                    P.act(sg[:], sg[:], AF.Exp, scale=-1.0)
                    yield
                    dst = (qT, kT, vT)[which][:, ci % 4, :]
                    dkey = [((qT, kT, vT)[which], ci % 4)]
                    if which == 2:
                        P.tt(dst, cy[:], sg[:], ALU.mult, outs=dkey)
                        yield
                    else:
                        P.tt(cy[:], cy[:], sg[:], ALU.mult)
                        yield
                        P.act(sq[:], cy[:], AF.Square)
                        yield
                        P.mm(ps2[:], lhsT=onesf[:], rhs=sq[:])
                        yield
                        P.act(rs[:], ps2[:], AF.Ln, bias=EPS, scale=1.0)
                        yield
                        P.act(rs[:], rs[:], AF.Exp, scale=-0.5)
                        yield
                        if which == 0:
                            P.stt(dst, cy[:], float(128 ** -0.5), rs[:], ALU.mult, ALU.mult, outs=dkey)
                            yield
                            P.copy(qTb[:, ci % 4, :], dst, eng="act", ins=dkey, outs=[(qTb, ci % 4)])
                        else:
                            P.tt(dst, cy[:], rs[:], ALU.mult, outs=dkey)
                            yield
                            P.copy(kTb[:, ci % 4, :], dst, eng="act", ins=dkey, outs=[(kTb, ci % 4)])
                        yield
                return gen

            facs = []
            if own:
                facs += [norm_chunk("qa", i) for i in range(4)]
            if own or halo:
                facs += [norm_chunk("ka", i) for i in range(4)]
            facs += [conv_chunk(ci) for ci in range(12) if (ci // 4 != 0 or own)]
            if own:
                facs += [norm_chunk("qm", i) for i in range(4)]
            pend = list(facs)
            active = {}
            for slot in range(2):
                if pend:
                    active[slot] = pend.pop(0)(slot)
            while active:
                for slot in sorted(active):
                    try:
                        next(active[slot])
                    except StopIteration:
                        if pend:
                            active[slot] = pend.pop(0)(slot)
                        else:
                            del active[slot]
            if own or halo:
                for ti in range(4):
                    ps = psb[3]
                    for kc in range(8):
                        P.mm(ps[:], lhsT=nT[:, kc, ti * 128:(ti + 1) * 128], rhs=wb[:, kc, 1024:1536],
                             start=(kc == 0), stop=(kc == 7), ins=[nT, (wb, (kc, 1024))])
                    v_ = vst[ti % 2]
                    P.copy(v_[:], ps[:], eng="act")
                    r0 = (tg + 512 if own else tg - 1536) + ti * 128
                    P.dma(S["va"][r0:r0 + 128, :], v_[:], outs=[(S["va"], r0)])
            if own:
                P.dma(S["nT"][:, :, tg:tg + 512].rearrange("k p t -> p k t"), nT[:], outs=[(S["nT"], tg)])
            def prep(cc):
                k = cc % 2
                cs = slice(cc * 64, (cc + 1) * 64)
                u_, wT, qkT, qdT, kdec, egl = u2[k], wT2[k], qkT2[k], qdT2[k], kdec2[k], egl2[k]
                sm = psb[0]
                for kc in range(8):
                    P.mm(sm[0:64, 0:8], lhsT=nT[:, kc, cs], rhs=wb[:, kc, 3584:3592],
                         start=(kc == 0), stop=(kc == 7), ins=[nT, (wb, (kc, 3072))], outs=[sm])
                P.act(bta[:], sm[0:64, 0:4], AF.Exp, scale=-1.0, ins=[sm])
                P.tt(gx[:], sm[0:64, 4:8], dtb[:], ALU.add, ins=[sm, dtb])
                yield
                P.act(bta[:], bta[:], AF.Ln, bias=1.0)
                P.act(gx[:], gx[:], AF.Exp)
                yield
                P.act(bta[:], bta[:], AF.Exp, scale=-1.0)
                P.act(gx[:], gx[:], AF.Ln, bias=1.0)
                yield
                P.ts(nbta[:], bta[:], -1.0, ALU.mult)
                P.tt(gg[:], gx[:], negA[:], ALU.mult)
                yield
                P.mm(sm[0:64, 8:12], lhsT=mU[:, 0, :], rhs=gg[:], outs=[sm])
                P.mm(sm[0:64, 12:16], lhsT=mSL[:, 0, :], rhs=gg[:], outs=[sm])
                P.mm(sm[:, 16:20], lhsT=onesf[0:64, :], rhs=gg[:], outs=[sm])
                P.tt(GU[:], mU[:], gg[:].unsqueeze(2).to_broadcast([64, 4, 64]), ALU.mult)
                yield
                P.act(egc[:], sm[0:64, 8:12], AF.Exp, ins=[sm])
                P.act(erev[:], sm[0:64, 12:16], AF.Exp, ins=[sm])
                P.act(egl[:], sm[:, 16:20], AF.Exp, ins=[sm])
                Dp = psb[6]
                Dv = Dp[0:64, 0:256].rearrange("p (h j) -> p h j", h=4)
                for h in range(4):
                    P.mm(Dv[:, h, :], lhsT=GU[:, h, :], rhs=onesf[0:64, 0:64], start=True, stop=False, outs=[Dp])
                    P.mm(Dv[:, h, :], lhsT=negonesf[0:64, 0:64], rhs=GU[:, h, :], start=False, stop=True, outs=[Dp])
                yield
                P.tt(bwe[:], bta[:], egc[:], ALU.mult)
                P.tt(EX[:, 0], Dv, mML[:], ALU.add, ins=[Dp, mML])
                if own:
                    P.stt(EX[:, 1], Dv, -1.0, mMU[:], ALU.mult, ALU.add, ins=[Dp, mMU])
                kP = psb[1]
                for h in range(4):
                    P.tr(kP[0:64, h * 128:(h + 1) * 128], kT[:, h, cs], identf[:], outs=[kP])
                kPv = kP[0:64, :].rearrange("p (h d) -> p h d", h=4)
                kk = psb[5]
                kkv = kk[0:64, :].rearrange("p (a h j) -> p a h j", a=2, h=4)
                for h in range(4):
                    P.mm(kkv[:, 0, h, :], lhsT=kTb[:, h, cs], rhs=kTb[:, h, cs], outs=[kk])
                    if own:
                        P.mm(kkv[:, 1, h, :], lhsT=kTb[:, h, cs], rhs=qTb[:, h, cs], outs=[kk])
                yield
                if own:
                    P.act(EX[:], EX[:], AF.Exp)
                else:
                    P.act(EX[:, 0], EX[:, 0], AF.Exp, ins=[EX], outs=[EX])
                P.tt(kbe[:], kPv, bwe[:].unsqueeze(2).to_broadcast([64, 4, 128]), ALU.mult, ins=[kP, bwe])
                P.tt(kdec[:], kPv, erev[:].unsqueeze(2).to_broadcast([64, 4, 128]), ALU.mult, ins=[kP, erev])
                yield
                vP = psb[1]
                for h in range(4):
                    P.tr(vP[0:64, h * 128:(h + 1) * 128], vT[:, h, cs], identf[:], outs=[vP])
                vPv = vP[0:64, :].rearrange("p (h d) -> p h d", h=4)
                P.tt(gamS[:], EX[:, 0], mS01[:], ALU.mult, ins=[EX, mS01])
                yield
                P.tt(vb_[:], vPv, bta[:].unsqueeze(2).to_broadcast([64, 4, 128]), ALU.mult, ins=[vP, bta])
                P.tt(Btmp[:], kkv[:, 0], gamS[:], ALU.mult, ins=[kk, gamS])
                yield
                P.tt(Bp[:, 0], Btmp[:], nbta[:].unsqueeze(2).to_broadcast([64, 4, 64]), ALU.mult,
                     ins=[Btmp, nbta], outs=[Bp])
                if own:
                    P.tt(qkT[:], kkv[:, 1], EX[:, 1], ALU.mult, ins=[kk, EX])
                    eb = psb[6]
                    P.mm(eb[:, 256:512], lhsT=onesf[0:64, :], rhs=GU[:].rearrange("p h j -> p (h j)"), outs=[eb])
                yield
                ctp = psb[4][:].bitcast(BF16)
                for h in range(4):
                    P.tr(ctp[0:64, h * 64:(h + 1) * 64], Bp[:, 0, h, :], ident[0:64, 0:64], outs=[psb[4]])
                if own:
                    P.act(egcB[:].rearrange("p h j -> p (h j)"), eb[:, 256:512], AF.Exp, ins=[eb])
                yield
                P.copy(Bp[:, 1], ctp[0:64, 0:256].rearrange("p (h j) -> p h j", h=4), eng="act",
                       ins=[psb[4]], outs=[Bp])
                if own:
                    P.tt(qdT[:], qT[:, :, cs], egcB[:], ALU.mult)
                yield
                P.tt(Pm[:], Bp[:, 1], mI[:], ALU.add, ins=[Bp, mI])
                cp = psb[3]
                cpv = cp[0:64, :].rearrange("p (a h j) -> p a h j", a=2, h=4)
                pu = psb[4]
                puv = pu[0:64, 0:256].rearrange("p (h j) -> p h j", h=4)
                for lvl in range(5):
                    last = lvl == 4
                    for h in range(4):
                        P.mm(cpv[:, 0, h, :], lhsT=Bp[:, 1, h, :], rhs=Bp[:, 0, h, :], outs=[cp])
                        if not last:
                            P.mm(cpv[:, 1, h, :], lhsT=Bp[:, 0, h, :], rhs=Bp[:, 1, h, :], outs=[cp])
                    yield
                    if last:
                        P.copy(Bp[:, 0], cpv[:, 0], eng="act", ins=[cp], outs=[Bp])
                    else:
                        P.copy(Bp[:], cpv, eng="act", ins=[cp], outs=[Bp])
                    yield
                    for h in range(4):
                        P.mm(puv[:, h, :], lhsT=Bp[:, 0, h, :], rhs=Pm[:, h, :], outs=[pu])
                    yield
                    P.tt(Pm[:], Pm[:], puv, ALU.add, ins=[Pm, pu])
                    yield
                up = psb[1]; wp = psb[5]
                upv = up[0:64, :].rearrange("p (h d) -> p h d", h=4)
                wpv = wp[:, 0:256].rearrange("p (h j) -> p h j", h=4)
                for h in range(4):
                    P.mm(upv[:, h, :], lhsT=Pm[:, h, :], rhs=vb_[:, h, :], outs=[up])
                    P.mm(wpv[:, h, :], lhsT=kbe[:, h, :], rhs=Pm[:, h, :], outs=[wp])
                yield
                P.copy(u_[:], upv, eng="act", ins=[up])
                P.copy(wT[:], wpv, eng="act", ins=[wp])
                yield

            def scan(cc):
                k = cc % 2
                cs = slice(cc * 64, (cc + 1) * 64)
                u_, wT, qkT, qdT, kdec, egl = u2[k], wT2[k], qkT2[k], qdT2[k], kdec2[k], egl2[k]
                ws = psb[7]
                wsv = ws[0:64, :].rearrange("p (h d) -> p h d", h=4)
                for h in range(4):
                    P.mm(wsv[:, h, :], lhsT=wT[:, h, :], rhs=Sb[:, h, :], outs=[ws])
                yield
                P.tt(vn[:], u_[:], wsv, ALU.subtract, ins=[u_, ws])
                yield
                if own:
                    op_ = psb[2]
                    opv = op_[0:64, :].rearrange("p (h d) -> p h d", h=4)
                    for h in range(4):
                        P.mm(opv[:, h, :], lhsT=qdT[:, h, :], rhs=Sb[:, h, :], start=True, stop=False, outs=[op_])
                        P.mm(opv[:, h, :], lhsT=qkT[:, h, :], rhs=vn[:, h, :], start=False, stop=True, outs=[op_])
                sn = psb[7]
                for h in range(4):
                    P.mm(sn[:, h * 128:(h + 1) * 128], lhsT=kdec[:, h, :], rhs=vn[:, h, :], outs=[sn])
                yield
                for h in range(4):
                    P.stt(Sst[:, h, :], Sst[:, h, :], egl[:, h:h + 1], sn[:, h * 128:(h + 1) * 128],
                          ALU.mult, ALU.add, ins=[Sst, egl, sn], outs=[Sst])
                yield
                P.copy(Sb[:], Sst[:], eng="act")
                if own:
                    P.act(osq[:], opv, AF.Square, ins=[op_])
                    yield
                    P.reduce(oss[:], osq[:])
                    yield
                    P.act(oss[:], oss[:], AF.Ln, bias=EPS, scale=1.0 / 128)
                    yield
                    P.act(ors[:], oss[:], AF.Exp, scale=-0.5)
                    yield
                    for h in range(4):
                        P.stt(on[:, h, :], opv[:, h, :], ors[:, h:h + 1], gob[:], ALU.mult, ALU.mult,
                              ins=[op_, ors, gob], outs=[(on, h)])
                    yield
                    zp = psb[2]
                    for kc in range(8):
                        P.mm(zp[0:64, :], lhsT=nT[:, kc, cs], rhs=wb[:, kc, 3072:3584],
                             start=(kc == 0), stop=(kc == 7), ins=[nT, (wb, (kc, 3072))], outs=[zp])
                    yield
                    P.act(sgz[:], zp[0:64, :], AF.Exp, scale=-1.0, ins=[zp])
                    yield
                    P.act(sgz[:], sgz[:], AF.Ln, bias=1.0)
                    yield
                    P.act(sgz[:], sgz[:], AF.Exp, scale=-1.0)
                    yield
                    P.tt(zs[:], zp[0:64, :], sgz[:], ALU.mult, ins=[zp, sgz])
                    yield
                    P.tt(obc[:], on[:].rearrange("p h d -> p (h d)"), zs[:], ALU.mult)
                    yield
                    tp = psb[7][:].bitcast(BF16)
                    for h in range(4):
                        P.tr(tp[:, h * 64:(h + 1) * 64], obc[:, h * 128:(h + 1) * 128], ident[0:64, 0:64],
                             outs=[psb[7]])
                    yield
                    P.copy(obT[:, :, cs], tp[:, 0:256].rearrange("p (h j) -> p h j", h=4), ins=[psb[7]])
                yield

            def interleave(g1, g2):
                gens = [g for g in (g1, g2) if g is not None]
                while gens:
                    for g in list(gens):
                        try:
                            next(g)
                        except StopIteration:
                            gens.remove(g)

            interleave(prep(0), None)
            for cc in range(8):
                interleave(scan(cc), prep(cc + 1) if cc + 1 < 8 else None)
            if own:
                for h in range(4):
                    P.dma(S["obT"][h, :, tg:tg + 512], obT[:, h, :], outs=[(S["obT"], (h, tg))])

        for b in range(4):
            block(I["xp"], b * 512, own=False, halo=(b == 3))
        for b in range(4):
            block(I["xo"], b * 512, own=True, halo=False)


def phase2(C):
    nc, P, I, S, K, psb = C.nc, C.P, C.I, C.S, C.K, C.psb
    onesb = K["onesb"]
    with ExitStack() as es:
        def sb(name, shape, dt=F32):
            return es.enter_context(nc.sbuf_tensor("p2_" + name, list(shape), dt))

        qaT = sb("qaT", [128, 4, T], BF16)
        kaT = sb("kaT", [128, 4, T + 512], BF16)
        va = sb("va", [64, 40, 512], BF16)
        ab = sb("ab", [64, 8, 576]); hm = sb("hm", [64, 8, 576])
        for ci in range(4):
            P.dma(qaT[:, ci, :], S["qaT"][ci])
            P.dma(kaT[:, ci, :], S["kaT"][ci])
        P.dma(va[:], S["va"].rearrange("(c p) f -> p c f", p=64))
        P.dma(ab[:], I["abias"]); P.dma(hm[:], I["hmask"])
        sbuf_s = [sb(f"s{i}", [64, 576]) for i in range(2)]
        pT = [sb(f"pT{i}", [64, 576], BF16) for i in range(2)]
        rden = sb("rden", [64, 512])
        oa = [sb(f"oa{i}", [64, 8, 64], BF16) for i in range(2)]
        pcb = ([sb(f"pc_u{i}", [128, 1024]) for i in range(8)], [sb(f"pc_v{i}", [128, 1024]) for i in range(8)],
               [sb(f"pc_ub{i}", [128, 1024], BF16) for i in range(4)],
               [sb(f"pc_vb{i}", [128, 1024], BF16) for i in range(4)])
        for i1 in range(4):
            precast_load(C, pcb, i1)

        def sc_mm(g):
            c, h = divmod(g, 8)
            hp, pb = h // 2, (h % 2) * 64
            s0, s1 = psb[(g % 2) * 2], psb[(g % 2) * 2 + 1]
            for i in range(9):
                dst = s0[0:64, i * 64:(i + 1) * 64] if i < 8 else s1[0:64, 0:64]
                P.mm(dst, lhsT=kaT[pb:pb + 64, hp, (c + i) * 64:(c + i + 1) * 64],
                     rhs=qaT[pb:pb + 64, hp, c * 64:(c + 1) * 64], outs=[s0 if i < 8 else s1])

        def post(g):
            c, h = divmod(g, 8)
            s0, s1 = psb[(g % 2) * 2], psb[(g % 2) * 2 + 1]
            s_ = sbuf_s[g % 2]; p_ = pT[g % 2]
            P.tt(s_[:, 0:512], s0[0:64, :], ab[:, h, 0:512], ALU.add, ins=[s0, ab], outs=[s_])
            P.tt(s_[:, 512:576], s1[0:64, 0:64], ab[:, h, 512:576], ALU.add, ins=[s1, ab], outs=[s_])
            if c < 8:
                P.tt(s_[:], s_[:], hm[:, c, :], ALU.add)
            P.act(p_[:], s_[:], AF.Exp)

        def ov_mm(g):
            c, h = divmod(g, 8)
            p_ = pT[g % 2]
            OT = psb[4 + 2 * (c % 2)]; DEN = psb[5 + 2 * (c % 2)]
            for i in range(9):
                P.mm(OT[0:64, h * 64:(h + 1) * 64], lhsT=va[:, c + i, h * 64:(h + 1) * 64],
                     rhs=p_[:, i * 64:(i + 1) * 64], start=(i == 0), stop=(i == 8), outs=[OT])
            for i in range(9):
                P.mm(DEN[0:64, h * 64:(h + 1) * 64], lhsT=onesb[0:64, 0:64],
                     rhs=p_[:, i * 64:(i + 1) * 64], start=(i == 0), stop=(i == 8), outs=[DEN])

        NG = 32 * 8
        sc_mm(0)
        for g in range(NG):
            c, h = divmod(g, 8)
            if h == 0 and c + 1 < 32:
                for i1 in range(4 * (c + 1), 4 * (c + 1) + 4):
                    precast_load(C, pcb, i1)
            if g + 1 < NG:
                sc_mm(g + 1)
            post(g)
            ov_mm(g)
            if h == 7:
                OT = psb[4 + 2 * (c % 2)]; DEN = psb[5 + 2 * (c % 2)]
                P.recip(rden[:], DEN[0:64, :])
                o_ = oa[c % 2]
                P.tt(o_[:].rearrange("p h q -> p (h q)"), OT[0:64, :], rden[:], ALU.mult)
                P.dma(S["oaT"][:, :, c * 64:(c + 1) * 64], o_[:], outs=[(S["oaT"], c)])
                for i1 in range(4 * c, 4 * c + 4):
                    precast_cast(C, pcb, i1)


def phase3(C):
    nc, P, I, S, K, psb = C.nc, C.P, C.I, C.S, C.K, C.psb
    ident, onesf, onesb = K["ident"], K["onesf"], K["onesb"]
    with ExitStack() as es:
        def sb(name, shape, dt=F32):
            return es.enter_context(nc.sbuf_tensor("p3_" + name, list(shape), dt))

        wkv = sb("wkv", [128, 8, D], BF16)
        stg = [sb(f"stg{i}", [128, D]) for i in range(2)]
        for kc in range(8):
            P.dma(stg[kc % 2][:], I["w_mem_kv"][kc * 128:(kc + 1) * 128, :])
            P.copy(wkv[:, kc, :], stg[kc % 2][:], eng=("act", "dve")[kc % 2], outs=[(wkv, kc)])
        gm = sb("gm", [128, D]); mkn = sb("mkn", [128, 1])
        P.dma(gm[:], I["g_mem"].broadcast_to([128, D])); P.dma(mkn[:], I["mkn"])
        junk = sb("junk", [128, D]); ss = sb("ss", [128, 1]); rstd = sb("rstd", [128, 1])
        mb = sb("mb", [128, D], BF16)
        memT = sb("memT", [128, 8, 256], BF16)
        for ti in range(2):
            x_ = stg[ti]
            P.dma(x_[:], I["mem"][ti * 128:(ti + 1) * 128, :])
            P.act(junk[:], x_[:], AF.Square, accum_out=ss[:])
            P.act(ss[:], ss[:], AF.Sqrt, bias=EPS, scale=1.0 / D)
            P.recip(rstd[:], ss[:])
            P.stt(mb[:], x_[:], rstd[:], gm[:], ALU.mult, ALU.mult)
            pT = psb[0][:].bitcast(BF16)
            for kc in range(8):
                P.tr(pT[:, kc * 128:(kc + 1) * 128], mb[:, kc * 128:(kc + 1) * 128], ident[:], outs=[psb[0]])
            P.copy(memT[:, :, ti * 128:(ti + 1) * 128], pT[:, 0:1024].rearrange("p (k t) -> p k t", k=8),
                   eng="act", ins=[psb[0]])
        kmT = sb("kmT", [128, 4, 256], BF16)
        vm = sb("vm", [128, 2, 512], BF16)
        sq = sb("sq", [128, 256]); rs = sb("rs", [128, 256])
        for h in range(4):
            ps = psb[1]; ps2 = psb[2]
            for kc in range(8):
                P.mm(ps[:, 0:256], lhsT=wkv[:, kc, h * 128:(h + 1) * 128], rhs=memT[:, kc, :],
                     start=(kc == 0), stop=(kc == 7), outs=[ps])
            P.act(sq[:], ps[:, 0:256], AF.Square, ins=[ps])
            P.mm(ps2[:, 0:256], lhsT=onesf[:], rhs=sq[:], outs=[ps2])
            P.act(rs[:], ps2[:, 0:256], AF.Sqrt, bias=EPS, scale=1.0 / 128, ins=[ps2])
            P.recip(rs[:], rs[:])
            P.stt(kmT[:, h, :], ps[:, 0:256], mkn[:], rs[:], ALU.mult, ALU.mult, ins=[ps, mkn, rs],
                  outs=[(kmT, h)])
        for ti in range(2):
            ps = psb[3]
            for kc in range(8):
                P.mm(ps[:], lhsT=memT[:, kc, ti * 128:(ti + 1) * 128], rhs=wkv[:, kc, 512:1024],
                     start=(kc == 0), stop=(kc == 7))
            P.copy(vm[:, ti, :], ps[:], eng="act", outs=[(vm, ti)])
        qm = [sb(f"qm{i}", [128, 512], BF16) for i in range(2)]
        pT_ = [sb(f"pT{i}", [128, 2, 512], BF16) for i in range(2)]
        rden = sb("rden", [128, 512])
        om = [sb(f"om{i}", [128, 512], BF16) for i in range(2)]
        n = 0
        for b in range(4):
            for h in range(4):
                q_ = qm[n % 2]; p_ = pT_[n % 2]; o_ = om[n % 2]
                P.dma(q_[:], S["qmT"][h, :, b * 512:(b + 1) * 512])
                for mt in range(2):
                    ps = psb[mt]
                    P.mm(ps[:], lhsT=kmT[:, h, mt * 128:(mt + 1) * 128], rhs=q_[:])
                    P.act(p_[:, mt, :], ps[:], AF.Exp, outs=[(p_, mt)])
                OT = psb[2 + 2 * (n % 2)]; DEN = psb[3 + 2 * (n % 2)]
                for mt in range(2):
                    P.mm(OT[:], lhsT=vm[:, mt, h * 128:(h + 1) * 128], rhs=p_[:, mt, :],
                         start=(mt == 0), stop=(mt == 1))
                for mt in range(2):
                    P.mm(DEN[:], lhsT=onesb[:], rhs=p_[:, mt, :], start=(mt == 0), stop=(mt == 1))
                P.recip(rden[:], DEN[:])
                P.tt(o_[:], OT[:], rden[:], ALU.mult)
                P.dma(S["omT"][h, :, b * 512:(b + 1) * 512], o_[:], outs=[(S["omT"], (h, b))])
                n += 1


def phase4(C):
    nc, P, I, S, K, psb = C.nc, C.P, C.I, C.S, C.K, C.psb
    ident, identf = K["ident"], K["identf"]
    with ExitStack() as es:
        def sb(name, shape, dt=F32):
            return es.enter_context(nc.sbuf_tensor("p4_" + name, list(shape), dt))

        iot = sb("iot", [128, 16, 16], BF16)
        P.op("pool", lambda e: e.iota(iot[:], pattern=[[0, 16], [1, 16]], base=0, channel_multiplier=0,
                                      allow_small_or_imprecise_dtypes=True), [], [iot])
        P.barrier()
        mg = sb("mg", [128, D]); h1 = sb("h1", [128, D])
        xt = sb("xt", [128, D])
        stg = [mg, h1, xt]
        wbrA = sb("wbrA", [64, 8, D], BF16)
        wbrB = sb("wbrB", [128, 4, D], BF16)
        wbrM = sb("wbrM", [128, 4, D], BF16)
        wo = sb("wo", [128, 8, D], BF16)
        wq = sb("wq", [128, 8, 2048], BF16)
        n = 0
        for h in range(8):
            st = stg[n % 3]
            P.dma(st[0:64, :], I["w_branch"][0, h * 64:(h + 1) * 64, :], outs=[st])
            P.copy(wbrA[:, h, :], st[0:64, :], eng=("act", "dve")[n % 2], ins=[st], outs=[(wbrA, h)])
            n += 1
        for bi, wt_ in ((1, wbrB), (2, wbrM)):
            for kc in range(4):
                st = stg[n % 3]
                P.dma(st[:], I["w_branch"][bi, kc * 128:(kc + 1) * 128, :])
                P.copy(wt_[:, kc, :], st[:], eng=("act", "dve")[n % 2], outs=[(wt_, kc)])
                n += 1
        for kc in range(8):
            st = stg[n % 3]
            P.dma(st[:], I["w_out"][kc * 128:(kc + 1) * 128, :])
            P.copy(wo[:, kc, :], st[:], eng=("act", "dve")[n % 2], outs=[(wo, kc)])
            n += 1
        for kc in range(8):
            for hf in range(2):
                st = stg[n % 3]
                P.dma(st[:], I["w_peer_q"][kc * 128:(kc + 1) * 128, hf * 1024:(hf + 1) * 1024])
                P.copy(wq[:, kc, hf * 1024:(hf + 1) * 1024], st[:], eng=("act", "dve")[n % 2],
                       outs=[(wq, (kc, hf))])
                n += 1
        wgt = sb("wgt", [128, 8, 3072], BF16)
        for kc in range(8):
            for g3 in range(3):
                st = stg[n % 3]
                P.dma(st[:], I["w_in"][kc * 128:(kc + 1) * 128, 4104 + g3 * 1024: 4104 + (g3 + 1) * 1024])
                P.copy(wgt[:, kc, g3 * 1024:(g3 + 1) * 1024], st[:], eng=("act", "dve")[n % 2],
                       outs=[(wgt, (kc, g3))])
                n += 1
        nTt = sb("nTt", [128, 8, 128], BF16)
        skT = sb("skT", [128, 2, 128])
        P.dma(skT[:], I["skT"])
        gf = sb("gf", [128, D])
        P.dma(gf[:], I["g_ffn"].broadcast_to([128, D]))

        oaT = sb("oaT", [64, 8, 128], BF16); obT = sb("obT", [128, 4, 128], BF16); omT = sb("omT", [128, 4, 128], BF16)
        gts = [sb(f"gts{i}", [128, 512]) for i in range(2)]
        mgb = sb("mgb", [128, D], BF16); mT = sb("mT", [128, 8, 128], BF16)
        ss = sb("ss", [128, 1]); rstd = sb("rstd", [128, 1])
        xnb = sb("xnb", [128, D], BF16); xnT = sb("xnT", [128, 8, 128], BF16)
        qTr = [sb(f"qT{i}", [128, 4, 128]) for i in range(2)]
        scr = [sb(f"sc{i}", [128, 16, 128]) for i in range(2)]
        sc2 = sb("sc2", [128, 128])
        v16 = sb("v16", [128, 16, 16]); ix = sb("ix", [128, 16, 16], U32); ixf = sb("ixf", [128, 16, 16])
        cand2 = sb("cand2", [128, 256])
        tv = sb("tv", [128, 8, 16]); pos = sb("pos", [128, 8, 16], U32)
        r1u = sb("r1u", [128, 8, 16], U32); r2u = sb("r2u", [128, 8, 16], U32)
        r1f = sb("r1f", [128, 8, 16], BF16); r2f = sb("r2f", [128, 8, 16], BF16)
        ixb = sb("ixb", [128, 16, 16], BF16)
        oh = sb("oh", [128, 8, 16, 16], BF16)
        i1f = sb("i1f", [128, 128]); i2f = sb("i2f", [128, 128])
        ge = sb("ge", [128, 8, 16]); gs = sb("gs", [128, 8]); gr = sb("gr", [128, 8]); gg = sb("gg", [128, 8, 16])
        trs = sb("trs", [128, 3, 128])
        pT = psb[2][:].bitcast(BF16)

        def partA(ti):
            sc = scr[ti % 2]
            ts_ = slice(ti * 128, (ti + 1) * 128)
            P.dma(oaT[:], S["oaT"][:, :, ts_])
            P.dma(obT[:], S["obT"][:, :, ts_].rearrange("k p t -> p k t"))
            P.dma(omT[:], S["omT"][:, :, ts_].rearrange("k p t -> p k t"))
            P.dma(nTt[:], S["nT"][:, :, ts_].rearrange("k p t -> p k t"))
            P.dma(xt[:], I["xo"][ts_, :])
            for br in range(3):
                for hf in range(2):
                    ps = psb[hf]
                    cs = slice(hf * 512, (hf + 1) * 512)
                    if br == 0:
                        for h in range(8):
                            P.mm(ps[:], lhsT=oaT[:, h, :], rhs=wbrA[:, h, cs], start=(h == 0), stop=(h == 7))
                    else:
                        src, w_ = (obT, wbrB) if br == 1 else (omT, wbrM)
                        for kc in range(4):
                            P.mm(ps[:], lhsT=src[:, kc, :], rhs=w_[:, kc, cs], start=(kc == 0), stop=(kc == 3))
                    gpsm = psb[4 + hf]
                    gco = br * 1024 + hf * 512
                    for kc in range(8):
                        P.mm(gpsm[:], lhsT=nTt[:, kc, :], rhs=wgt[:, kc, gco:gco + 512],
                             start=(kc == 0), stop=(kc == 7))
                    gts_ = gts[hf]
                    P.act(gts_[:], gpsm[:], AF.Sigmoid)
                    gsl = gts_[:]
                    if br == 0:
                        P.tt(mg[:, cs], ps[:], gsl, ALU.mult, ins=[ps, gts_], outs=[(mg, hf)])
                        yield
                    else:
                        P.tt(gts_[:], ps[:], gsl, ALU.mult, ins=[ps, gts_], outs=[gts_])
                        P.tt(mg[:, cs], mg[:, cs], gts_[:], ALU.add, eng="dve", ins=[(mg, hf), gts_],
                             outs=[(mg, hf)])
                    yield
            P.copy(mgb[:], mg[:], eng="act")
            yield
            for kc in range(8):
                P.tr(pT[:, kc * 128:(kc + 1) * 128], mgb[:, kc * 128:(kc + 1) * 128], ident[:], outs=[psb[2]])
            P.copy(mT[:], pT[:, 0:1024].rearrange("p (k t) -> p k t", k=8), eng="act", ins=[psb[2]])
            yield
            for hf in range(2):
                ps = psb[hf]
                cs = slice(hf * 512, (hf + 1) * 512)
                for kc in range(8):
                    P.mm(ps[:], lhsT=mT[:, kc, :], rhs=wo[:, kc, cs], start=(kc == 0), stop=(kc == 7))
                P.tt(h1[:, cs], ps[:], xt[:, cs], ALU.add, ins=[ps, xt], outs=[(h1, hf)])
                yield
            P.dma(S["h1"][ts_, :], h1[:], outs=[(S["h1"], ti)])
            P.act(xnb[:], h1[:], AF.Square, accum_out=ss[:])
            yield
            P.act(ss[:], ss[:], AF.Sqrt, bias=EPS, scale=1.0 / D)
            yield
            P.recip(rstd[:], ss[:])
            yield
            P.stt(xnb[:], h1[:], rstd[:], gf[:], ALU.mult, ALU.mult)
            yield
            for kc in range(8):
                P.tr(pT[:, kc * 128:(kc + 1) * 128], xnb[:, kc * 128:(kc + 1) * 128], ident[:], outs=[psb[2]])
            P.copy(xnT[:], pT[:, 0:1024].rearrange("p (k t) -> p k t", k=8), eng="act", ins=[psb[2]])
            yield
            P.dma(S["xn2T"][:, :, ts_].rearrange("k p t -> p k t"), xnT[:], outs=[(S["xn2T"], ti)])
            for g4 in range(4):
                ps = psb[2]
                qT = qTr[g4 % 2]
                for j in range(4):
                    ch = g4 * 4 + j
                    for kc in range(8):
                        P.mm(ps[:, j * 128:(j + 1) * 128], lhsT=wq[:, kc, ch * 128:(ch + 1) * 128],
                             rhs=xnT[:, kc, :], start=(kc == 0), stop=(kc == 7), outs=[ps])
                P.copy(qT[:], ps[:].rearrange("p (j t) -> p j t", j=4), eng="act", ins=[ps])
                yield
                ps2 = psb[4 + g4]
                for j in range(4):
                    ch = g4 * 4 + j
                    P.mm(ps2[:, j * 128:(j + 1) * 128], lhsT=qT[:, j, :], rhs=skT[:, ch % 2, :], outs=[ps2])
                P.copy(sc[:, g4 * 4:(g4 + 1) * 4, :], ps2[:].rearrange("p (j t) -> p j t", j=4),
                       eng="act", ins=[ps2], outs=[(sc, g4)])
                yield

        def partB(ti):
            sc = scr[ti % 2]
            ts_ = slice(ti * 128, (ti + 1) * 128)
            cand = sc[:].rearrange("p a b -> p (a b)").rearrange("p (h c) -> p h c", h=8)
            for hp in range(16):
                k_ = (sc, hp // 4)
                P.op("dve", lambda e, hp=hp: e.max(out=v16[:, hp, 0:8], in_=sc[:, hp, :]), [k_], [(v16, hp)])
                P.op("dve", lambda e, hp=hp: e.max_index(out=ix[:, hp, 0:8], in_max=v16[:, hp, 0:8],
                                                          in_values=sc[:, hp, :]), [k_, (v16, hp)], [(ix, hp)])
                P.op("dve", lambda e, hp=hp: e.match_replace(out=sc2[:], in_to_replace=v16[:, hp, 0:8],
                                                              in_values=sc[:, hp, :], imm_value=-1e30),
                     [k_, (v16, hp)], [sc2])
                yield
                P.op("dve", lambda e, hp=hp: e.max(out=v16[:, hp, 8:16], in_=sc2[:]), [sc2], [(v16, hp)])
                P.op("dve", lambda e, hp=hp: e.max_index(out=ix[:, hp, 8:16], in_max=v16[:, hp, 8:16],
                                                          in_values=sc2[:]), [sc2, (v16, hp)], [(ix, hp)])
                yield
            P.copy(ixf[:], ix[:])
            P.copy(ixb[:], ix[:], eng="act")
            v5 = v16[:].rearrange("p (h a) r -> p h a r", a=2)
            x5 = ixf[:].rearrange("p (h a) r -> p h a r", a=2)
            for h in range(8):
                P.tt(cand[:, h, :].rearrange("p (a b) -> p a b", a=16),
                     v5[:, h, 0, :].unsqueeze(2).to_broadcast([128, 16, 16]),
                     v5[:, h, 1, :].unsqueeze(1).to_broadcast([128, 16, 16]), ALU.add,
                     ins=[v16], outs=[sc])
                if h % 2 == 1:
                    yield
            for h in range(8):
                k_ = sc
                P.op("dve", lambda e, h=h: e.max(out=tv[:, h, 0:8], in_=cand[:, h, :]), [k_], [(tv, h)])
                P.op("dve", lambda e, h=h: e.max_index(out=pos[:, h, 0:8], in_max=tv[:, h, 0:8],
                                                        in_values=cand[:, h, :]), [k_, (tv, h)], [(pos, h)])
                P.op("dve", lambda e, h=h: e.match_replace(out=cand2[:], in_to_replace=tv[:, h, 0:8],
                                                            in_values=cand[:, h, :], imm_value=-1e30),
                     [k_, (tv, h)], [cand2])
                yield
                P.op("dve", lambda e, h=h: e.max(out=tv[:, h, 8:16], in_=cand2[:]), [cand2], [(tv, h)])
                P.op("dve", lambda e, h=h: e.max_index(out=pos[:, h, 8:16], in_max=tv[:, h, 8:16],
                                                        in_values=cand2[:]), [cand2, (tv, h)], [(pos, h)])
                yield
            P.ts(r1u[:], pos[:], 4, ALU.logical_shift_right)
            P.ts(r2u[:], pos[:], 15, ALU.bitwise_and)
            P.copy(r1f[:], r1u[:]); P.copy(r2f[:], r2u[:])
            xb5 = ixb[:].rearrange("p (h a) r -> p h a r", a=2)
            for a_, rf, dst in ((0, r1f, i1f), (1, r2f, i2f)):
                P.tt(oh[:], iot[:].unsqueeze(1).to_broadcast([128, 8, 16, 16]),
                     rf[:].unsqueeze(3).to_broadcast([128, 8, 16, 16]), ALU.is_equal)
                P.tt(oh[:], oh[:], xb5[:, :, a_, :].unsqueeze(2).to_broadcast([128, 8, 16, 16]), ALU.mult,
                     ins=[oh, ixb])
                P.reduce(dst[:], oh[:].rearrange("p h j r -> p (h j) r"))
                yield
            P.tt(ge[:], tv[:], tv[:, :, 0:1].to_broadcast([128, 8, 16]), ALU.subtract)
            P.act(ge[:], ge[:], AF.Exp)
            P.reduce(gs[:], ge[:])
            P.recip(gr[:], gs[:])
            P.tt(gg[:], ge[:], gr[:].unsqueeze(2).to_broadcast([128, 8, 16]), ALU.mult)
            yield
            tp = psb[3]
            P.tr(tp[:, 0:128], i1f[:], identf[:], outs=[tp])
            P.tr(tp[:, 128:256], i2f[:], identf[:], outs=[tp])
            P.tr(tp[:, 256:384], gg[:].rearrange("p h j -> p (h j)"), identf[:], outs=[tp])
            P.copy(trs[:], tp[:, 0:384].rearrange("p (a t) -> p a t", a=3), eng="act", ins=[tp])
            P.dma(S["i1T"][:, ts_], trs[:, 0, :], ins=[trs], outs=[(S["i1T"], ti)])
            P.dma(S["i2T"][:, ts_], trs[:, 1, :], ins=[trs], outs=[(S["i2T"], ti)])
            P.dma(S["gT"][:, ts_], trs[:, 2, :], ins=[trs], outs=[(S["gT"], ti)])
            yield

        def interleave(g1, g2, r1=1, r2=1):
            gens = [[g, r] for g, r in ((g1, r1), (g2, r2)) if g is not None]
            while gens:
                for gr_ in list(gens):
                    try:
                        for _ in range(gr_[1]):
                            next(gr_[0])
                    except StopIteration:
                        gens.remove(gr_)

        interleave(partA(0), None)
        for ti in range(16):
            interleave(partA(ti + 1) if ti + 1 < 16 else None, partB(ti), 1, 3)


TB = 256


def precast_load(C, bufs, i1):
    P, I = C.P, C.I
    ust, vst, ubs, vbs = bufs
    P.dma(ust[i1 % 8][:], I["utr"][i1])
    P.dma(vst[i1 % 8][:], I["peer_v"][i1 * 128:(i1 + 1) * 128, :])


def precast_cast(C, bufs, i1):
    P, S = C.P, C.S
    ust, vst, ubs, vbs = bufs
    ub_, vb_ = ubs[i1 % 4], vbs[i1 % 4]
    P.copy(ub_[:], ust[i1 % 8][:], eng="act")
    P.copy(vb_[:], vst[i1 % 8][:], eng="dve")
    P.dma(S["ub"][i1], ub_[:], outs=[(S["ub"], i1)])
    P.dma(S["vb"][i1 * 128:(i1 + 1) * 128, :], vb_[:], outs=[(S["vb"], i1)])


def phase5(C):
    nc, P, I, S, K, psb = C.nc, C.P, C.I, C.S, C.K, C.psb
    with ExitStack() as es:
        def sb(name, shape, dt=F32):
            return es.enter_context(nc.sbuf_tensor("p5_" + name, list(shape), dt))

        iob = sb("iob", [128, 128], BF16)
        P.op("pool", lambda e: e.iota(iob[:], pattern=[[1, 128]], base=0, channel_multiplier=0,
                                      allow_small_or_imprecise_dtypes=True), [], [iob])
        P.barrier()
        NB = T // TB
        WTs = [sb(f"WT{i}", [128, TB, 128], BF16) for i in range(2)]
        i1Ts = [sb(f"i1T{i}", [128, TB]) for i in range(2)]
        i2Ts = [sb(f"i2T{i}", [128, TB]) for i in range(2)]
        gTs = [sb(f"gT{i}", [128, TB]) for i in range(2)]
        xnTs = [sb(f"xnT{i}", [128, 8, TB], BF16) for i in range(2)]
        At = [sb(f"At{i}", [128, 128], BF16) for i in range(4)]
        Bt = [sb(f"Bt{i}", [128, 128], BF16) for i in range(4)]
        NR = 8
        ub = [sb(f"ub{i}", [128, 8, 128], BF16) for i in range(NR)]
        vb = [sb(f"vb{i}", [128, 1024], BF16) for i in range(NR)]
        ga = [sb(f"ga{i}", [128, TB], BF16) for i in range(2)]
        wg = [sb(f"wg{i}", [128, TB], BF16) for i in range(2)]
        h1 = sb("h1", [128, D]); ot = sb("ot", [128, D])

        def load_blk(blk):
            bs = slice(blk * TB, (blk + 1) * TB)
            k = blk % 2
            P.dma(i1Ts[k][:], S["i1T"][:, bs]); P.dma(i2Ts[k][:], S["i2T"][:, bs]); P.dma(gTs[k][:], S["gT"][:, bs])
            P.dma(xnTs[k][:], S["xn2T"][:, :, bs].rearrange("k p t -> p k t"))

        def wbuild(blk, t):
            k = blk % 2
            WT, i1T, i2T, gT = WTs[k], i1Ts[k], i2Ts[k], gTs[k]
            a_, b_ = At[t % 4], Bt[t % 4]
            P.ts(a_[:], iob[:], i1T[:, t:t + 1], ALU.is_equal, ins=[iob, i1T])
            P.ts(b_[:], iob[:], i2T[:, t:t + 1], ALU.is_equal, gT[:, t:t + 1], ALU.mult,
                 ins=[iob, i2T, gT])
            wp = psb[4]
            P.mm(wp[:, (t % 4) * 128:(t % 4 + 1) * 128], lhsT=b_[:], rhs=a_[:], outs=[wp])
            if t % 4 == 3:
                t0 = t - 3
                P.copy(WT[:, t0:t0 + 4, :],
                       wp[:].rearrange("p (t i) -> p t i", t=4), eng="act", ins=[wp],
                       outs=[(WT, t0)])

        load_blk(0)
        for t in range(TB):
            wbuild(0, t)
        OP = [[psb[0], psb[1]], [psb[2], psb[3]]]
        for blk in range(NB):
            k = blk % 2
            WT, xnT = WTs[k], xnTs[k]
            if blk + 1 < NB:
                load_blk(blk + 1)

            def ld(i1):
                ub_, vb_ = ub[i1 % NR], vb[i1 % NR]
                P.dma(ub_[:].rearrange("p k e -> p (k e)"), S["ub"][i1], ins=[(S["ub"], i1)])
                P.dma(vb_[:], S["vb"][i1 * 128:(i1 + 1) * 128, :], ins=[(S["vb"], i1)])

            def a_mm(i1):
                ub_ = ub[i1 % NR]
                ap_ = psb[5 + i1 % 3]
                for kc in range(8):
                    P.mm(ap_[:, 0:TB], lhsT=ub_[:, kc, :], rhs=xnT[:, kc, :], start=(kc == 0), stop=(kc == 7),
                         outs=[ap_])

            def o_mm(i1):
                vb_ = vb[i1 % NR]
                ap_ = psb[5 + i1 % 3]
                g_ = ga[i1 % 2]; w_ = wg[i1 % 2]
                P.act(g_[:], ap_[:, 0:TB], AF.Gelu, ins=[ap_])
                P.tt(w_[:], g_[:], WT[:, :, i1], ALU.mult, ins=[g_, WT])
                for sub in range(TB // 128):
                    for hf in range(2):
                        P.mm(OP[sub][hf][:], lhsT=w_[:, sub * 128:(sub + 1) * 128],
                             rhs=vb_[:, hf * 512:(hf + 1) * 512], start=(i1 == 0), stop=(i1 == 127))

            for j in range(NR - 1):
                ld(j)
            a_mm(0)
            a_mm(1)
            for i1 in range(128):
                if i1 + NR - 1 < 128:
                    ld(i1 + NR - 1)
                if i1 + 2 < 128:
                    a_mm(i1 + 2)
                o_mm(i1)
                if blk + 1 < NB:
                    for t in range(2 * i1, 2 * i1 + 2):
                        wbuild(blk + 1, t)
            for sub in range(TB // 128):
                r0 = blk * TB + sub * 128
                P.dma(h1[:], S["h1"][r0:r0 + 128, :])
                for hf in range(2):
                    P.tt(ot[:, hf * 512:(hf + 1) * 512], OP[sub][hf][:], h1[:, hf * 512:(hf + 1) * 512], ALU.add,
                         ins=[OP[sub][hf], h1], outs=[(ot, hf)])
                P.dma(C.out[r0:r0 + 128, :], ot[:], outs=[(C.out, r0)])


_NC = None


def _prep(inp):
    f = np.float32
    x = np.asarray(inp["x"], f)
    mem = np.asarray(inp["mem"], f)
    rb = np.asarray(inp["a_rel_bias"], f)[0]
    kk = np.arange(64)[:, None, None]
    ii = np.arange(9)[None, :, None]
    qq = np.arange(64)[None, None, :]
    rel = qq - ((ii - 8) * 64 + kk)
    idx = np.clip(rel, -128, 128) + 128
    abias = np.ascontiguousarray(rb[:, idx].transpose(1, 0, 2, 3).reshape(64, 8, 576))
    convw = np.ascontiguousarray(np.asarray(inp["b_conv"], f)[0, :, 0, :].reshape(4, 12, 128).transpose(2, 1, 0))
    pu = np.asarray(inp["peer_u"], f)[0]
    utr = np.ascontiguousarray(pu.reshape(128, 128, 8, 128).transpose(0, 3, 2, 1).reshape(128, 128, 1024))
    skT = np.ascontiguousarray(np.asarray(inp["peer_sub_keys"], f)[0].transpose(2, 0, 1))
    common = {
        "g_mix": np.asarray(inp["g_mix_norm"], f).reshape(1, D),
        "w_in": np.ascontiguousarray(np.asarray(inp["w_in"], f)[0]),
        "aqn": np.tile(np.asarray(inp["a_q_norm"], f)[0], 2).reshape(128, 1),
        "akn": np.tile(np.asarray(inp["a_k_norm"], f)[0], 2).reshape(128, 1),
        "abias": abias,
        "convw": convw,
        "b_a_log": np.asarray(inp["b_a_log"], f).reshape(1, 4),
        "b_dt_bias": np.asarray(inp["b_dt_bias"], f).reshape(1, 4),
        "b_out_norm": np.asarray(inp["b_out_norm"], f).reshape(1, 128),
        "g_mem": np.asarray(inp["g_mem_norm"], f).reshape(1, D),
        "w_mem_kv": np.ascontiguousarray(np.asarray(inp["w_mem_kv"], f)[0]),
        "mqn": np.asarray(inp["m_q_norm"], f).reshape(128, 1),
        "mkn": np.asarray(inp["m_k_norm"], f).reshape(128, 1),
        "w_branch": np.ascontiguousarray(np.asarray(inp["w_branch"], f)[0]),
        "w_out": np.ascontiguousarray(np.asarray(inp["w_out"], f)[0]),
        "g_ffn": np.asarray(inp["g_ffn_norm"], f).reshape(1, D),
        "w_peer_q": np.ascontiguousarray(np.asarray(inp["w_peer_q"], f)[0]),
        "skT": skT,
        "utr": utr,
        "peer_v": np.ascontiguousarray(np.asarray(inp["peer_v"], f)[0]),
    }
    hm0 = np.zeros((64, 8, 576), f)
    for c in range(8):
        hm0[:, c, :(8 - c) * 64] = NEG
    hm1 = np.zeros((64, 8, 576), f)
    zeros = np.zeros((T, D), f)
    maps = []
    for core in range(8):
        b, half = core // 2, core % 2
        m = dict(common)
        m["xo"] = np.ascontiguousarray(x[b, half * T:(half + 1) * T])
        m["xp"] = np.ascontiguousarray(x[b, 0:T]) if half == 1 else zeros
        m["mem"] = np.ascontiguousarray(mem[b])
        m["hmask"] = hm0 if half == 0 else hm1
        maps.append(m)
    return maps


def kernel(**inputs):
    global _NC
    if _NC is None:
        _NC = build_nc()
    maps = _prep(inputs)
    res = run_bass_kernel_spmd(_NC, maps, core_ids=list(range(8)))
    out = np.zeros((4, 2 * T, D), np.float32)
    for core in range(8):
        b, half = core // 2, core % 2
        out[b, half * T:(half + 1) * T] = np.asarray(res.results[core]["out"], np.float32)
    return out
```

```python
from contextlib import ExitStack
import numpy as np
import ml_dtypes
import concourse.bass as bass
import concourse.mybir as mybir
from concourse.bass_utils import run_bass_kernel_spmd

F32 = mybir.dt.float32
BF16 = mybir.dt.bfloat16
U32 = mybir.dt.uint32
I32 = mybir.dt.int32
AF = mybir.ActivationFunctionType
ALU = mybir.AluOpType
AX = mybir.AxisListType

N_DMA_SEMS = 24
ENGS = ("sp", "act", "pool", "pe", "dve")
EPS = 1e-6
NEG = -30000.0

T = 2048
D = 1024
INC = 7176


class Op:
    __slots__ = ("eng", "fn", "deps", "dma", "needed", "sig", "idx", "presem")

    def __init__(self, eng, fn, dma):
        self.eng = eng
        self.fn = fn
        self.dma = dma
        self.deps = []
        self.needed = False
        self.sig = None
        self.presem = None


def _nm(x):
    sub = None
    if isinstance(x, tuple):
        x, sub = x
    t = getattr(x, "tensor", None)
    name = t.name if t is not None else x.name
    return name, sub


class Prog:
    def __init__(self, nc):
        self.nc = nc
        self.ops = []
        self.st = {}
        self.last_on = {}
        self.dma_since = []

    def _deps_r(self, name, sub, deps):
        s = self.st.get(name)
        if s is None:
            return
        w = s["w"]
        if sub is None:
            deps.update(w.values())
        else:
            if sub in w:
                deps.add(w[sub])
            if None in w:
                deps.add(w[None])

    def _deps_w(self, name, sub, deps):
        self._deps_r(name, sub, deps)
        s = self.st.get(name)
        if s is None:
            return
        r = s["r"]
        if sub is None:
            for l in r.values():
                deps.update(l)
        else:
            deps.update(r.get(sub, ()))
            deps.update(r.get(None, ()))

    def op(self, eng, fn, ins=(), outs=(), dma=False):
        o = Op(eng, fn, dma)
        o.idx = len(self.ops)
        deps = set()
        rk = [_nm(x) for x in ins]
        wk = [_nm(x) for x in outs]
        for n, s in rk:
            self._deps_r(n, s, deps)
        for n, s in wk:
            self._deps_w(n, s, deps)
        o.deps = sorted(deps)
        for n, s in rk:
            st = self.st.setdefault(n, {"w": {}, "r": {}})
            st["r"].setdefault(s, []).append(o.idx)
        for n, s in wk:
            st = self.st.setdefault(n, {"w": {}, "r": {}})
            if s is None:
                st["w"] = {None: o.idx}
                st["r"] = {}
            else:
                st["w"][s] = o.idx
                st["r"][s] = []
        self.ops.append(o)
        self.last_on[eng] = o.idx
        if dma:
            self.dma_since.append(o.idx)
        return o

    def barrier(self):
        deps = sorted(set(list(self.last_on.values()) + self.dma_since))
        self.dma_since = []
        new = []
        for e in ENGS:
            o = Op(e, lambda eng: eng.nop(), False)
            o.idx = len(self.ops)
            o.deps = [d for d in deps]
            self.ops.append(o)
            new.append(o.idx)
        for e, i in zip(ENGS, new):
            self.last_on[e] = i
        self.st = {}

    def dma(self, out, in_, ins=None, outs=None, eng="sp"):
        return self.op(eng, lambda e: e.dma_start(out=out, in_=in_),
                       [in_] if ins is None else ins, [out] if outs is None else outs, dma=True)

    def mm(self, out, lhsT, rhs, start=True, stop=True, ins=None, outs=None):
        i = [lhsT, rhs] if ins is None else ins
        return self.op("pe", lambda e: e.matmul(out, lhsT=lhsT, rhs=rhs, start=start, stop=stop),
                       i, [out] if outs is None else outs)

    def tr(self, out, in_, ident, ins=None, outs=None):
        return self.op("pe", lambda e: e.transpose(out=out, in_=in_, identity=ident),
                       [in_, ident] if ins is None else ins, [out] if outs is None else outs)

    def act(self, out, in_, func, bias=None, scale=None, accum_out=None, ins=None, outs=None):
        kw = {}
        i = [in_]
        if bias is not None:
            kw["bias"] = bias
            if not isinstance(bias, (int, float)):
                i.append(bias)
        if scale is not None:
            kw["scale"] = scale
            if not isinstance(scale, (int, float)):
                i.append(scale)
        o = [out]
        if accum_out is not None:
            kw["accum_out"] = accum_out
            o.append(accum_out)
        return self.op("act", lambda e: e.activation(out=out, in_=in_, func=func, **kw),
                       i if ins is None else ins, o if outs is None else outs)

    def tt(self, out, in0, in1, op, eng="dve", ins=None, outs=None):
        return self.op(eng, lambda e: e.tensor_tensor(out=out, in0=in0, in1=in1, op=op),
                       [in0, in1] if ins is None else ins, [out] if outs is None else outs)

    def ts(self, out, in0, s1, op0, s2=None, op1=None, eng="dve", accum_out=None, ins=None, outs=None):
        i = [in0]
        for s in (s1, s2):
            if s is not None and not isinstance(s, (int, float)):
                i.append(s)
        kw = {}
        if op1 is not None:
            kw["op1"] = op1
        o = [out]
        if accum_out is not None:
            kw["accum_out"] = accum_out
            o.append(accum_out)
        return self.op(eng, lambda e: e.tensor_scalar(out=out, in0=in0, scalar1=s1, scalar2=s2, op0=op0, **kw),
                       i if ins is None else ins, o if outs is None else outs)

    def stt(self, out, in0, scalar, in1, op0, op1, accum_out=None, ins=None, outs=None):
        i = [in0, in1]
        if not isinstance(scalar, (int, float)):
            i.append(scalar)
        kw = {}
        o = [out]
        if accum_out is not None:
            kw["accum_out"] = accum_out
            o.append(accum_out)
        return self.op("dve", lambda e: e.scalar_tensor_tensor(out=out, in0=in0, scalar=scalar, in1=in1,
                                                               op0=op0, op1=op1, **kw),
                       i if ins is None else ins, o if outs is None else outs)

    def copy(self, out, in_, eng="dve", ins=None, outs=None):
        if eng == "act":
            f = lambda e: e.copy(out=out, in_=in_)
        else:
            f = lambda e: e.tensor_copy(out=out, in_=in_)
        return self.op(eng, f, [in_] if ins is None else ins, [out] if outs is None else outs)

    def recip(self, out, in_):
        return self.op("dve", lambda e: e.reciprocal(out=out, in_=in_), [in_], [out])

    def memset(self, out, val, eng="pool"):
        return self.op(eng, lambda e: e.memset(out, val), [], [out])

    def reduce(self, out, in_, op=ALU.add, axis=AX.X):
        return self.op("dve", lambda e: e.tensor_reduce(out=out, in_=in_, axis=axis, op=op), [in_], [out])

    def emit(self, sems, dma_sems):
        ops = self.ops
        for o in ops:
            latest = {}
            keep = []
            for d in o.deps:
                od = ops[d]
                if od.dma:
                    keep.append(d)
                else:
                    if od.eng == "pe" and o.eng == "pe" and not o.dma:
                        continue
                    if od.eng not in latest or latest[od.eng] < d:
                        latest[od.eng] = d
            keep.extend(latest.values())
            o.deps = sorted(keep)
            for d in o.deps:
                ops[d].needed = True
        cnt = {e: 0 for e in ENGS}
        nds = len(dma_sems)
        dma_use = [0] * nds
        rr = 0
        for o in ops:
            if o.dma:
                s = rr % nds
                rr += 1
                if dma_use[s] > 0:
                    o.presem = (dma_sems[s], 16 * dma_use[s])
                dma_use[s] += 1
                o.sig = (dma_sems[s], 16 * dma_use[s])
            elif o.needed:
                cnt[o.eng] += 1
                o.sig = (sems[o.eng], cnt[o.eng])
        per_eng = {e: [o for o in ops if o.eng == e] for e in ENGS}
        final_dma = [(dma_sems[s], 16 * dma_use[s]) for s in range(nds) if dma_use[s] > 0]
        nc = self.nc

        def run(eng_name, e):
            waited = {}

            def wait(sem, val):
                k = id(sem)
                if waited.get(k, 0) >= val:
                    return
                waited[k] = val
                e.wait_ge(sem, val)

            for o in per_eng[eng_name]:
                for d in o.deps:
                    sem, val = ops[d].sig
                    wait(sem, val)
                if o.presem is not None:
                    wait(*o.presem)
                ins = o.fn(e)
                if o.sig is not None:
                    sem, val = o.sig
                    ins.then_inc(sem, 16 if o.dma else 1)
            if eng_name == "sp":
                for sem, val in final_dma:
                    wait(sem, val)

        with nc.Block() as block:
            @block.sync
            def _(e):
                run("sp", e)

            @block.scalar
            def _(e):
                run("act", e)

            @block.gpsimd
            def _(e):
                run("pool", e)

            @block.tensor
            def _(e):
                run("pe", e)

            @block.vector
            def _(e):
                run("dve", e)


class Ctx:
    pass


def build_nc(dbg=()):
    nc = bass.Bass("TRN2", target_bir_lowering=False)
    P = Prog(nc)
    C = Ctx()
    C.nc, C.P = nc, P

    def din(name, shape, dt=F32):
        return nc.dram_tensor(name, list(shape), dt, kind="ExternalInput").ap()

    def dscr(name, shape, dt=F32):
        kind = "ExternalOutput" if name in dbg else "Internal"
        return nc.dram_tensor(name, list(shape), dt, kind=kind).ap()

    I = C.I = {}
    I["xo"] = din("xo", [T, D])
    I["xp"] = din("xp", [T, D])
    I["mem"] = din("mem", [256, D])
    I["g_mix"] = din("g_mix", [1, D])
    I["w_in"] = din("w_in", [D, INC])
    I["aqn"] = din("aqn", [128, 1])
    I["akn"] = din("akn", [128, 1])
    I["abias"] = din("abias", [64, 8, 576])
    I["hmask"] = din("hmask", [64, 8, 576])
    I["convw"] = din("convw", [128, 12, 4])
    I["b_a_log"] = din("b_a_log", [1, 4])
    I["b_dt_bias"] = din("b_dt_bias", [1, 4])
    I["b_out_norm"] = din("b_out_norm", [1, 128])
    I["g_mem"] = din("g_mem", [1, D])
    I["w_mem_kv"] = din("w_mem_kv", [D, D])
    I["mqn"] = din("mqn", [128, 1])
    I["mkn"] = din("mkn", [128, 1])
    I["w_branch"] = din("w_branch", [3, 512, D])
    I["w_out"] = din("w_out", [D, D])
    I["g_ffn"] = din("g_ffn", [1, D])
    I["w_peer_q"] = din("w_peer_q", [D, 2048])
    I["skT"] = din("skT", [128, 2, 128])
    I["utr"] = din("utr", [128, 128, 1024])
    I["peer_v"] = din("peer_v", [16384, D])
    out = C.out = nc.dram_tensor("out", [T, D], F32, kind="ExternalOutput").ap()

    S = C.S = {}
    S["qaT"] = dscr("qaT", [4, 128, T], BF16)
    S["kaT"] = dscr("kaT", [4, 128, T + 512], BF16)
    S["va"] = dscr("va", [T + 512, 512], BF16)
    S["qmT"] = dscr("qmT", [4, 128, T], BF16)
    S["obT"] = dscr("obT", [4, 128, T], BF16)
    S["nT"] = dscr("nT", [8, 128, T], BF16)
    S["oaT"] = dscr("oaT", [64, 8, T], BF16)
    S["omT"] = dscr("omT", [4, 128, T], BF16)
    S["h1"] = dscr("h1", [T, D], F32)
    S["xn2T"] = dscr("xn2T", [8, 128, T], BF16)
    S["i1T"] = dscr("i1T", [128, T], F32)
    S["i2T"] = dscr("i2T", [128, T], F32)
    S["gT"] = dscr("gT", [128, T], F32)
    S["ub"] = dscr("ub", [128, 128, 1024], BF16)
    S["vb"] = dscr("vb", [16384, D], BF16)

    with ExitStack() as top:
        sems = {e: top.enter_context(nc.semaphore("s_" + e)) for e in ENGS}
        dsems = [top.enter_context(nc.semaphore(f"dq{i}")) for i in range(N_DMA_SEMS)]

        def gsb(name, shape, dt):
            return top.enter_context(nc.sbuf_tensor(name, list(shape), dt))

        K = C.K = {}
        identf = K["identf"] = gsb("identf", [128, 128], F32)
        ident = K["ident"] = gsb("ident", [128, 128], BF16)
        onesf = K["onesf"] = gsb("onesf", [128, 128], F32)
        negonesf = K["negonesf"] = gsb("negonesf", [128, 128], F32)
        onesb = K["onesb"] = gsb("onesb", [128, 128], BF16)
        bd64 = K["bd64"] = gsb("bd64", [128, 128], F32)
        P.memset(onesf[:], 1.0)
        P.memset(negonesf[:], -1.0)
        P.memset(identf[:], 1.0)
        P.op("pool", lambda e: e.affine_select(out=identf[:], in_=identf[:], pattern=[[-1, 128]],
                                               compare_op=ALU.is_equal, fill=0.0, base=0,
                                               channel_multiplier=1), [identf], [identf])
        P.copy(ident[:], identf[:], eng="act")
        P.copy(onesb[:], onesf[:], eng="act")
        P.memset(bd64[:], 0.0)
        P.memset(bd64[0:64, 0:64], 1.0)
        P.memset(bd64[64:128, 64:128], 1.0)

        psb = [top.enter_context(nc.psum_tensor(f"pb{i}", [128, 512], F32)) for i in range(8)]
        C.psb = psb

        phase1(C)
        P.barrier()
        phase2(C)
        P.barrier()
        phase3(C)
        P.barrier()
        phase4(C)
        P.barrier()
        phase5(C)
        P.emit(sems, dsems)
    return nc


def _mask_tile(C, es, name, cmp_lower_incl=None, kind=None):
    nc, P = C.nc, C.P
    t = es.enter_context(nc.sbuf_tensor(name, [64, 4, 64], F32))
    if kind == "U":
        P.memset(t[:], 1.0)
        args = dict(pattern=[[0, 4], [1, 64]], compare_op=ALU.is_ge, fill=0.0, base=0, channel_multiplier=-1)
    elif kind == "SL":
        P.memset(t[:], 1.0)
        args = dict(pattern=[[0, 4], [-1, 64]], compare_op=ALU.is_gt, fill=0.0, base=0, channel_multiplier=1)
    elif kind == "ML":
        P.memset(t[:], 0.0)
        args = dict(pattern=[[0, 4], [-1, 64]], compare_op=ALU.is_ge, fill=NEG, base=0, channel_multiplier=1)
    elif kind == "MU":
        P.memset(t[:], 0.0)
        args = dict(pattern=[[0, 4], [1, 64]], compare_op=ALU.is_ge, fill=NEG, base=0, channel_multiplier=-1)
    elif kind == "S01":
        P.memset(t[:], 1.0)
        args = dict(pattern=[[0, 4], [-1, 64]], compare_op=ALU.is_gt, fill=0.0, base=0, channel_multiplier=1)
    elif kind == "I":
        P.memset(t[:], 1.0)
        args = dict(pattern=[[0, 4], [-1, 64]], compare_op=ALU.is_equal, fill=0.0, base=0, channel_multiplier=1)
    P.op("pool", lambda e: e.affine_select(out=t[:], in_=t[:], **args), [t], [t])
    return t


def phase1(C):
    nc, P, I, S, K, psb = C.nc, C.P, C.I, C.S, C.K, C.psb
    identf, ident, onesf, negonesf, onesb, bd64 = (K[k] for k in
                                                   ("identf", "ident", "onesf", "negonesf", "onesb", "bd64"))
    with ExitStack() as es:
        def sb(name, shape, dt=F32):
            return es.enter_context(nc.sbuf_tensor("p1_" + name, list(shape), dt))

        mU = _mask_tile(C, es, "p1_mU", kind="U")
        mSL = _mask_tile(C, es, "p1_mSL", kind="SL")
        mML = _mask_tile(C, es, "p1_mML", kind="ML")
        mMU = _mask_tile(C, es, "p1_mMU", kind="MU")
        mS01 = _mask_tile(C, es, "p1_mS01", kind="S01")
        mI = _mask_tile(C, es, "p1_mI", kind="I")

        Sst = sb("Sst", [128, 4, 128])
        P.memset(Sst[:], 0.0)
        Sb = sb("Sb", [128, 4, 128], BF16)
        P.memset(Sb[:], 0.0)
        hist = sb("hist", [128, 12, 3])
        P.memset(hist[:], 0.0)
        P.barrier()

        WC = 4104
        wb = sb("wb", [128, 8, WC], BF16)
        xt = [sb(f"xt{i}", [128, D]) for i in range(2)]
        stg = xt
        n = 0
        for c0 in (2048, 3072, 4096, 0, 1024):
            for kc in range(8):
                w = min(1024, WC - c0)
                st = stg[n % 2]
                P.dma(st[:, 0:w], I["w_in"][kc * 128:(kc + 1) * 128, c0:c0 + w])
                P.copy(wb[:, kc, c0:c0 + w], st[:, 0:w], eng=("act", "dve")[n % 2],
                       outs=[(wb, (kc, c0))])
                n += 1
        gt = sb("gt", [128, D])
        P.dma(gt[:], I["g_mix"].broadcast_to([128, D]))
        aqn = sb("aqn", [128, 1]); akn = sb("akn", [128, 1]); mqn = sb("mqn", [128, 1])
        P.dma(aqn[:], I["aqn"]); P.dma(akn[:], I["akn"]); P.dma(mqn[:], I["mqn"])
        P.ts(aqn[:], aqn[:], 0.125, ALU.mult)
        P.ts(mqn[:], mqn[:], float(128 ** -0.5), ALU.mult)
        convw = sb("convw", [128, 12, 4])
        P.dma(convw[:], I["convw"])
        dtb = sb("dtb", [64, 4]); negA = sb("negA", [64, 4]); gob = sb("gob", [64, 128])
        P.dma(dtb[:], I["b_dt_bias"].broadcast_to([64, 4]))
        P.dma(negA[:], I["b_a_log"].broadcast_to([64, 4]))
        P.dma(gob[:], I["b_out_norm"].broadcast_to([64, 128]))
        P.act(negA[:], negA[:], AF.Exp)
        P.ts(negA[:], negA[:], -1.0, ALU.mult)
        nbf = [sb(f"nbf{i}", [128, D], BF16) for i in range(2)]
        ss = sb("ss", [128, 1]); rstd = sb("rstd", [128, 1])
        nT = sb("nT", [128, 8, 512], BF16)
        sqr = [sb(f"sq{i}", [128, 512]) for i in range(2)]
        rsr = [sb(f"rs{i}", [128, 512]) for i in range(2)]
        obf = [sb(f"obf{i}", [128, 512], BF16) for i in range(2)]
        cbr = [sb(f"cb{i}", [128, 515]) for i in range(2)]
        cyr = [sb(f"cy{i}", [128, 512]) for i in range(2)]
        sgr = [sb(f"sg{i}", [128, 512]) for i in range(2)]
        qT = sb("qT", [128, 4, 512]); kT = sb("kT", [128, 4, 512]); vT = sb("vT", [128, 4, 512])
        qTb = sb("qTb", [128, 4, 512], BF16); kTb = sb("kTb", [128, 4, 512], BF16)
        vst = [sb(f"vst{i}", [128, 512], BF16) for i in range(2)]
        obT = sb("obT", [128, 4, 512], BF16)
        bta = sb("bta", [64, 4]); nbta = sb("nbta", [64, 4]); bwe = sb("bwe", [64, 4])
        gx = sb("gx", [64, 4]); gg = sb("gg", [64, 4])
        egc = sb("egc", [64, 4]); erev = sb("erev", [64, 4]); egl = sb("egl", [128, 4])
        GU = sb("GU", [64, 4, 64])
        EX = sb("EX", [64, 2, 4, 64])
        gamS = sb("gamS", [64, 4, 64])
        egcB = sb("egcB", [128, 4, 64])
        qdT = sb("qdT", [128, 4, 64])
        vb_ = sb("vb_", [64, 4, 128], BF16); kbe = sb("kbe", [64, 4, 128], BF16); kdec = sb("kdec", [64, 4, 128])
        Bp = sb("Bp", [64, 2, 4, 64], BF16)
        Btmp = sb("Btmp", [64, 4, 64])
        Pm = sb("Pm", [64, 4, 64], BF16)
        sgz = sb("sgz", [64, 512])
        qkT = sb("qkT", [64, 4, 64])
        u2 = [sb(f"u_{i}", [64, 4, 128]) for i in range(2)]; wT2 = [sb(f"wT{i}", [128, 4, 64], BF16) for i in range(2)]
        vn = sb("vn", [64, 4, 128], BF16)
        qkT2 = [sb(f"qkT{i}", [64, 4, 64], BF16) for i in range(2)]
        qdT2 = [sb(f"qdT{i}", [128, 4, 64], BF16) for i in range(2)]
        kdec2 = [sb(f"kdec{i}", [64, 4, 128], BF16) for i in range(2)]; egl2 = [sb(f"egl{i}", [128, 4]) for i in range(2)]
        osq = sb("osq", [64, 4, 128]); oss = sb("oss", [64, 4]); ors = sb("ors", [64, 4])
        on = sb("on", [64, 4, 128]); zs = sb("zs", [64, 512]); obc = sb("obc", [64, 512], BF16)

        def rms_stats(src_ps, npart):
            pass

        def chan_proj(co, ps):
            for kc in range(8):
                wk = [(wb, (kc, (co // 1024) * 1024))]
                if (co + 127) // 1024 != co // 1024:
                    wk.append((wb, (kc, ((co + 127) // 1024) * 1024)))
                P.mm(ps[:], lhsT=wb[:, kc, co:co + 128], rhs=nT[:, kc, :], start=(kc == 0), stop=(kc == 7),
                     ins=wk + [nT])

        evi = [0]

        def block(xsrc, t0, own, halo):
            for ti in range(4):
                x_ = xt[ti % 2]; nb = nbf[ti % 2]
                P.dma(x_[:], xsrc[t0 + ti * 128: t0 + (ti + 1) * 128, :])
                P.act(nb[:], x_[:], AF.Square, accum_out=ss[:])
                P.act(ss[:], ss[:], AF.Ln, bias=EPS, scale=1.0 / D)
                P.act(rstd[:], ss[:], AF.Exp, scale=-0.5)
                P.stt(nb[:], x_[:], rstd[:], gt[:], ALU.mult, ALU.mult)
                pT = psb[0][:].bitcast(BF16)
                for kc in range(8):
                    P.tr(pT[:, kc * 128:(kc + 1) * 128], nb[:, kc * 128:(kc + 1) * 128], ident[:],
                         outs=[psb[0]])
                P.copy(nT[:, :, ti * 128:(ti + 1) * 128],
                       pT[:, 0:1024].rearrange("p (k t) -> p k t", k=8), eng="act", ins=[psb[0]])
            tg = t0
            def norm_chunk(kind, ci):
                def gen(e_):
                    ps = psb[1 + 2 * e_]; ps2 = psb[2 + 2 * e_]; sq = sqr[e_]; rs = rsr[e_]; ob = obf[e_]
                    if kind == "qa":
                        co, gain, lhs, sc_ = ci * 128, aqn, bd64, 1.0 / 64
                    elif kind == "ka":
                        co, gain, lhs, sc_ = 512 + ci * 128, akn, bd64, 1.0 / 64
                    else:
                        co, gain, lhs, sc_ = 3592 + ci * 128, mqn, onesf, 1.0 / 128
                    chan_proj(co, ps)
                    yield
                    P.act(sq[:], ps[:], AF.Square)
                    yield
                    P.mm(ps2[:], lhsT=lhs[:], rhs=sq[:])
                    yield
                    P.act(rs[:], ps2[:], AF.Ln, bias=EPS, scale=sc_)
                    yield
                    P.act(rs[:], rs[:], AF.Exp, scale=-0.5)
                    yield
                    P.stt(ob[:], ps[:], gain[:], rs[:], ALU.mult, ALU.mult)
                    yield
                    if kind == "qa":
                        P.dma(S["qaT"][ci, :, tg:tg + 512], ob[:], outs=[(S["qaT"], (ci, tg))])
                    elif kind == "ka":
                        kt = tg + 512 if own else tg - 1536
                        P.dma(S["kaT"][ci, :, kt:kt + 512], ob[:], outs=[(S["kaT"], (ci, kt))])
                    else:
                        P.dma(S["qmT"][ci, :, tg:tg + 512], ob[:], outs=[(S["qmT"], (ci, tg))])
                    yield
                return gen

            def conv_chunk(ci):
                def gen(e_):
                    which = ci // 4
                    co = 1536 + ci * 128
                    ps = psb[1 + 2 * e_]; ps2 = psb[2 + 2 * e_]; sq = sqr[e_]; rs = rsr[e_]
                    cb = cbr[e_]; cy = cyr[e_]; sg = sgr[e_]
                    chan_proj(co, ps)
                    P.copy(cb[:, 0:3], hist[:, ci, :], eng="act")
                    yield
                    P.copy(cb[:, 3:515], ps[:], eng="dve")
                    yield
                    P.copy(hist[:, ci, :], cb[:, 512:515], eng="act")
                    P.ts(cy[:], cb[:, 0:512], convw[:, ci, 0:1], ALU.mult)
                    yield
                    for w_ in range(1, 4):
                        P.stt(cy[:], cb[:, w_:w_ + 512], convw[:, ci, w_:w_ + 1], cy[:], ALU.mult, ALU.add)
                        yield
                    P.act(sg[:], cy[:], AF.Exp, scale=-1.0)
                    yield
                    P.act(sg[:], sg[:], AF.Ln, bias=1.0)
                    yield
                    P.act(sg[:], sg[:], AF.Exp, scale=-1.0)
                    yield
                    dst = (qT, kT, vT)[which][:, ci % 4, :]
                    dkey = [((qT, kT, vT)[which], ci % 4)]
                    if which == 2:
                        P.tt(dst, cy[:], sg[:], ALU.mult, outs=dkey)
                        yield
                    else:
                        P.tt(cy[:], cy[:], sg[:], ALU.mult)
                        yield
                        P.act(sq[:], cy[:], AF.Square)
                        yield
                        P.mm(ps2[:], lhsT=onesf[:], rhs=sq[:])
                        yield
                        P.act(rs[:], ps2[:], AF.Ln, bias=EPS, scale=1.0)
                        yield
                        P.act(rs[:], rs[:], AF.Exp, scale=-0.5)
                        yield
                        if which == 0:
                            P.stt(dst, cy[:], float(128 ** -0.5), rs[:], ALU.mult, ALU.mult, outs=dkey)
                            yield
                            P.copy(qTb[:, ci % 4, :], dst, eng="act", ins=dkey, outs=[(qTb, ci % 4)])
                        else:
                            P.tt(dst, cy[:], rs[:], ALU.mult, outs=dkey)
                            yield
                            P.copy(kTb[:, ci % 4, :], dst, eng="act", ins=dkey, outs=[(kTb, ci % 4)])
                        yield
                return gen

            facs = []
            if own:
                facs += [norm_chunk("qa", i) for i in range(4)]
            if own or halo:
                facs += [norm_chunk("ka", i) for i in range(4)]
            facs += [conv_chunk(ci) for ci in range(12) if (ci // 4 != 0 or own)]
            if own:
                facs += [norm_chunk("qm", i) for i in range(4)]
            pend = list(facs)
            active = {}
            for slot in range(2):
                if pend:
                    active[slot] = pend.pop(0)(slot)
            while active:
                for slot in sorted(active):
                    try:
                        next(active[slot])
                    except StopIteration:
                        if pend:
                            active[slot] = pend.pop(0)(slot)
                        else:
                            del active[slot]
            if own or halo:
                for ti in range(4):
                    ps = psb[3]
                    for kc in range(8):
                        P.mm(ps[:], lhsT=nT[:, kc, ti * 128:(ti + 1) * 128], rhs=wb[:, kc, 1024:1536],
                             start=(kc == 0), stop=(kc == 7), ins=[nT, (wb, (kc, 1024))])
                    v_ = vst[ti % 2]
                    P.copy(v_[:], ps[:], eng="act")
                    r0 = (tg + 512 if own else tg - 1536) + ti * 128
                    P.dma(S["va"][r0:r0 + 128, :], v_[:], outs=[(S["va"], r0)])
            if own:
                P.dma(S["nT"][:, :, tg:tg + 512].rearrange("k p t -> p k t"), nT[:], outs=[(S["nT"], tg)])
            def prep(cc):
                k = cc % 2
                cs = slice(cc * 64, (cc + 1) * 64)
                u_, wT, qkT, qdT, kdec, egl = u2[k], wT2[k], qkT2[k], qdT2[k], kdec2[k], egl2[k]
                sm = psb[0]
                for kc in range(8):
                    P.mm(sm[0:64, 0:8], lhsT=nT[:, kc, cs], rhs=wb[:, kc, 3584:3592],
                         start=(kc == 0), stop=(kc == 7), ins=[nT, (wb, (kc, 3072))], outs=[sm])
                P.act(bta[:], sm[0:64, 0:4], AF.Exp, scale=-1.0, ins=[sm])
                P.tt(gx[:], sm[0:64, 4:8], dtb[:], ALU.add, ins=[sm, dtb])
                yield
                P.act(bta[:], bta[:], AF.Ln, bias=1.0)
                P.act(gx[:], gx[:], AF.Exp)
                yield
                P.act(bta[:], bta[:], AF.Exp, scale=-1.0)
                P.act(gx[:], gx[:], AF.Ln, bias=1.0)
                yield
                P.ts(nbta[:], bta[:], -1.0, ALU.mult)
                P.tt(gg[:], gx[:], negA[:], ALU.mult)
                yield
                P.mm(sm[0:64, 8:12], lhsT=mU[:, 0, :], rhs=gg[:], outs=[sm])
                P.mm(sm[0:64, 12:16], lhsT=mSL[:, 0, :], rhs=gg[:], outs=[sm])
                P.mm(sm[:, 16:20], lhsT=onesf[0:64, :], rhs=gg[:], outs=[sm])
                P.tt(GU[:], mU[:], gg[:].unsqueeze(2).to_broadcast([64, 4, 64]), ALU.mult)
                yield
                P.act(egc[:], sm[0:64, 8:12], AF.Exp, ins=[sm])
                P.act(erev[:], sm[0:64, 12:16], AF.Exp, ins=[sm])
                P.act(egl[:], sm[:, 16:20], AF.Exp, ins=[sm])
                Dp = psb[6]
                Dv = Dp[0:64, 0:256].rearrange("p (h j) -> p h j", h=4)
                for h in range(4):
                    P.mm(Dv[:, h, :], lhsT=GU[:, h, :], rhs=onesf[0:64, 0:64], start=True, stop=False, outs=[Dp])
                    P.mm(Dv[:, h, :], lhsT=negonesf[0:64, 0:64], rhs=GU[:, h, :], start=False, stop=True, outs=[Dp])
                yield
                P.tt(bwe[:], bta[:], egc[:], ALU.mult)
                P.tt(EX[:, 0], Dv, mML[:], ALU.add, ins=[Dp, mML])
                if own:
                    P.stt(EX[:, 1], Dv, -1.0, mMU[:], ALU.mult, ALU.add, ins=[Dp, mMU])
                kP = psb[1]
                for h in range(4):
                    P.tr(kP[0:64, h * 128:(h + 1) * 128], kT[:, h, cs], identf[:], outs=[kP])
                kPv = kP[0:64, :].rearrange("p (h d) -> p h d", h=4)
                kk = psb[5]
                kkv = kk[0:64, :].rearrange("p (a h j) -> p a h j", a=2, h=4)
                for h in range(4):
                    P.mm(kkv[:, 0, h, :], lhsT=kTb[:, h, cs], rhs=kTb[:, h, cs], outs=[kk])
                    if own:
                        P.mm(kkv[:, 1, h, :], lhsT=kTb[:, h, cs], rhs=qTb[:, h, cs], outs=[kk])
                yield
                if own:
                    P.act(EX[:], EX[:], AF.Exp)
                else:
                    P.act(EX[:, 0], EX[:, 0], AF.Exp, ins=[EX], outs=[EX])
                P.tt(kbe[:], kPv, bwe[:].unsqueeze(2).to_broadcast([64, 4, 128]), ALU.mult, ins=[kP, bwe])
                P.tt(kdec[:], kPv, erev[:].unsqueeze(2).to_broadcast([64, 4, 128]), ALU.mult, ins=[kP, erev])
                yield
                vP = psb[1]
                for h in range(4):
                    P.tr(vP[0:64, h * 128:(h + 1) * 128], vT[:, h, cs], identf[:], outs=[vP])
                vPv = vP[0:64, :].rearrange("p (h d) -> p h d", h=4)
                P.tt(gamS[:], EX[:, 0], mS01[:], ALU.mult, ins=[EX, mS01])
                yield
                P.tt(vb_[:], vPv, bta[:].unsqueeze(2).to_broadcast([64, 4, 128]), ALU.mult, ins=[vP, bta])
                P.tt(Btmp[:], kkv[:, 0], gamS[:], ALU.mult, ins=[kk, gamS])
                yield
                P.tt(Bp[:, 0], Btmp[:], nbta[:].unsqueeze(2).to_broadcast([64, 4, 64]), ALU.mult,
                     ins=[Btmp, nbta], outs=[Bp])
                if own:
                    P.tt(qkT[:], kkv[:, 1], EX[:, 1], ALU.mult, ins=[kk, EX])
                    eb = psb[6]
                    P.mm(eb[:, 256:512], lhsT=onesf[0:64, :], rhs=GU[:].rearrange("p h j -> p (h j)"), outs=[eb])
                yield
                ctp = psb[4][:].bitcast(BF16)
                for h in range(4):
                    P.tr(ctp[0:64, h * 64:(h + 1) * 64], Bp[:, 0, h, :], ident[0:64, 0:64], outs=[psb[4]])
                if own:
                    P.act(egcB[:].rearrange("p h j -> p (h j)"), eb[:, 256:512], AF.Exp, ins=[eb])
                yield
                P.copy(Bp[:, 1], ctp[0:64, 0:256].rearrange("p (h j) -> p h j", h=4), eng="act",
                       ins=[psb[4]], outs=[Bp])
                if own:
                    P.tt(qdT[:], qT[:, :, cs], egcB[:], ALU.mult)
                yield
                P.tt(Pm[:], Bp[:, 1], mI[:], ALU.add, ins=[Bp, mI])
                cp = psb[3]
                cpv = cp[0:64, :].rearrange("p (a h j) -> p a h j", a=2, h=4)
                pu = psb[4]
                puv = pu[0:64, 0:256].rearrange("p (h j) -> p h j", h=4)
                for lvl in range(5):
                    last = lvl == 4
                    for h in range(4):
                        P.mm(cpv[:, 0, h, :], lhsT=Bp[:, 1, h, :], rhs=Bp[:, 0, h, :], outs=[cp])
                        if not last:
                            P.mm(cpv[:, 1, h, :], lhsT=Bp[:, 0, h, :], rhs=Bp[:, 1, h, :], outs=[cp])
                    yield
                    if last:
                        P.copy(Bp[:, 0], cpv[:, 0], eng="act", ins=[cp], outs=[Bp])
                    else:
                        P.copy(Bp[:], cpv, eng="act", ins=[cp], outs=[Bp])
                    yield
                    for h in range(4):
                        P.mm(puv[:, h, :], lhsT=Bp[:, 0, h, :], rhs=Pm[:, h, :], outs=[pu])
                    yield
                    P.tt(Pm[:], Pm[:], puv, ALU.add, ins=[Pm, pu])
                    yield
                up = psb[1]; wp = psb[5]
                upv = up[0:64, :].rearrange("p (h d) -> p h d", h=4)
                wpv = wp[:, 0:256].rearrange("p (h j) -> p h j", h=4)
                for h in range(4):
                    P.mm(upv[:, h, :], lhsT=Pm[:, h, :], rhs=vb_[:, h, :], outs=[up])
                    P.mm(wpv[:, h, :], lhsT=kbe[:, h, :], rhs=Pm[:, h, :], outs=[wp])
                yield
                P.copy(u_[:], upv, eng="act", ins=[up])
                P.copy(wT[:], wpv, eng="act", ins=[wp])
                yield

            def scan(cc):
                k = cc % 2
                cs = slice(cc * 64, (cc + 1) * 64)
                u_, wT, qkT, qdT, kdec, egl = u2[k], wT2[k], qkT2[k], qdT2[k], kdec2[k], egl2[k]
                ws = psb[7]
                wsv = ws[0:64, :].rearrange("p (h d) -> p h d", h=4)
                for h in range(4):
                    P.mm(wsv[:, h, :], lhsT=wT[:, h, :], rhs=Sb[:, h, :], outs=[ws])
                yield
                P.tt(vn[:], u_[:], wsv, ALU.subtract, ins=[u_, ws])
                yield
                if own:
                    op_ = psb[2]
                    opv = op_[0:64, :].rearrange("p (h d) -> p h d", h=4)
                    for h in range(4):
                        P.mm(opv[:, h, :], lhsT=qdT[:, h, :], rhs=Sb[:, h, :], start=True, stop=False, outs=[op_])
                        P.mm(opv[:, h, :], lhsT=qkT[:, h, :], rhs=vn[:, h, :], start=False, stop=True, outs=[op_])
                sn = psb[7]
                for h in range(4):
                    P.mm(sn[:, h * 128:(h + 1) * 128], lhsT=kdec[:, h, :], rhs=vn[:, h, :], outs=[sn])
                yield
                for h in range(4):
                    P.stt(Sst[:, h, :], Sst[:, h, :], egl[:, h:h + 1], sn[:, h * 128:(h + 1) * 128],
                          ALU.mult, ALU.add, ins=[Sst, egl, sn], outs=[Sst])
                yield
                P.copy(Sb[:], Sst[:], eng="act")
                if own:
                    P.act(osq[:], opv, AF.Square, ins=[op_])
                    yield
                    P.reduce(oss[:], osq[:])
                    yield
                    P.act(oss[:], oss[:], AF.Ln, bias=EPS, scale=1.0 / 128)
                    yield
                    P.act(ors[:], oss[:], AF.Exp, scale=-0.5)
                    yield
                    for h in range(4):
                        P.stt(on[:, h, :], opv[:, h, :], ors[:, h:h + 1], gob[:], ALU.mult, ALU.mult,
                              ins=[op_, ors, gob], outs=[(on, h)])
                    yield
                    zp = psb[2]
                    for kc in range(8):
                        P.mm(zp[0:64, :], lhsT=nT[:, kc, cs], rhs=wb[:, kc, 3072:3584],
                             start=(kc == 0), stop=(kc == 7), ins=[nT, (wb, (kc, 3072))], outs=[zp])
                    yield
                    P.act(sgz[:], zp[0:64, :], AF.Exp, scale=-1.0, ins=[zp])
                    yield
                    P.act(sgz[:], sgz[:], AF.Ln, bias=1.0)
                    yield
                    P.act(sgz[:], sgz[:], AF.Exp, scale=-1.0)
                    yield
                    P.tt(zs[:], zp[0:64, :], sgz[:], ALU.mult, ins=[zp, sgz])
                    yield
                    P.tt(obc[:], on[:].rearrange("p h d -> p (h d)"), zs[:], ALU.mult)
                    yield
                    tp = psb[7][:].bitcast(BF16)
                    for h in range(4):
                        P.tr(tp[:, h * 64:(h + 1) * 64], obc[:, h * 128:(h + 1) * 128], ident[0:64, 0:64],
                             outs=[psb[7]])
                    yield
                    P.copy(obT[:, :, cs], tp[:, 0:256].rearrange("p (h j) -> p h j", h=4), ins=[psb[7]])
                yield

            def interleave(g1, g2):
                gens = [g for g in (g1, g2) if g is not None]
                while gens:
                    for g in list(gens):
                        try:
                            next(g)
                        except StopIteration:
                            gens.remove(g)

            interleave(prep(0), None)
            for cc in range(8):
                interleave(scan(cc), prep(cc + 1) if cc + 1 < 8 else None)
            if own:
                for h in range(4):
                    P.dma(S["obT"][h, :, tg:tg + 512], obT[:, h, :], outs=[(S["obT"], (h, tg))])

        for b in range(4):
            block(I["xp"], b * 512, own=False, halo=(b == 3))
        for b in range(4):
            block(I["xo"], b * 512, own=True, halo=False)


def phase2(C):
    nc, P, I, S, K, psb = C.nc, C.P, C.I, C.S, C.K, C.psb
    onesb = K["onesb"]
    with ExitStack() as es:
        def sb(name, shape, dt=F32):
            return es.enter_context(nc.sbuf_tensor("p2_" + name, list(shape), dt))

        qaT = sb("qaT", [128, 4, T], BF16)
        kaT = sb("kaT", [128, 4, T + 512], BF16)
        va = sb("va", [64, 40, 512], BF16)
        ab = sb("ab", [64, 8, 576]); hm = sb("hm", [64, 8, 576])
        for ci in range(4):
            P.dma(qaT[:, ci, :], S["qaT"][ci])
            P.dma(kaT[:, ci, :], S["kaT"][ci])
        P.dma(va[:], S["va"].rearrange("(c p) f -> p c f", p=64))
        P.dma(ab[:], I["abias"]); P.dma(hm[:], I["hmask"])
        sbuf_s = [sb(f"s{i}", [64, 576]) for i in range(2)]
        pT = [sb(f"pT{i}", [64, 576], BF16) for i in range(2)]
        rden = sb("rden", [64, 512])
        oa = [sb(f"oa{i}", [64, 8, 64], BF16) for i in range(2)]
        pcb = ([sb(f"pc_u{i}", [128, 1024]) for i in range(8)], [sb(f"pc_v{i}", [128, 1024]) for i in range(8)],
               [sb(f"pc_ub{i}", [128, 1024], BF16) for i in range(4)],
               [sb(f"pc_vb{i}", [128, 1024], BF16) for i in range(4)])
        for i1 in range(4):
            precast_load(C, pcb, i1)

        def sc_mm(g):
            c, h = divmod(g, 8)
            hp, pb = h // 2, (h % 2) * 64
            s0, s1 = psb[(g % 2) * 2], psb[(g % 2) * 2 + 1]
            for i in range(9):
                dst = s0[0:64, i * 64:(i + 1) * 64] if i < 8 else s1[0:64, 0:64]
                P.mm(dst, lhsT=kaT[pb:pb + 64, hp, (c + i) * 64:(c + i + 1) * 64],
                     rhs=qaT[pb:pb + 64, hp, c * 64:(c + 1) * 64], outs=[s0 if i < 8 else s1])

        def post(g):
            c, h = divmod(g, 8)
            s0, s1 = psb[(g % 2) * 2], psb[(g % 2) * 2 + 1]
            s_ = sbuf_s[g % 2]; p_ = pT[g % 2]
            P.tt(s_[:, 0:512], s0[0:64, :], ab[:, h, 0:512], ALU.add, ins=[s0, ab], outs=[s_])
            P.tt(s_[:, 512:576], s1[0:64, 0:64], ab[:, h, 512:576], ALU.add, ins=[s1, ab], outs=[s_])
            if c < 8:
                P.tt(s_[:], s_[:], hm[:, c, :], ALU.add)
            P.act(p_[:], s_[:], AF.Exp)

        def ov_mm(g):
            c, h = divmod(g, 8)
            p_ = pT[g % 2]
            OT = psb[4 + 2 * (c % 2)]; DEN = psb[5 + 2 * (c % 2)]
            for i in range(9):
                P.mm(OT[0:64, h * 64:(h + 1) * 64], lhsT=va[:, c + i, h * 64:(h + 1) * 64],
                     rhs=p_[:, i * 64:(i + 1) * 64], start=(i == 0), stop=(i == 8), outs=[OT])
            for i in range(9):
                P.mm(DEN[0:64, h * 64:(h + 1) * 64], lhsT=onesb[0:64, 0:64],
                     rhs=p_[:, i * 64:(i + 1) * 64], start=(i == 0), stop=(i == 8), outs=[DEN])

        NG = 32 * 8
        sc_mm(0)
        for g in range(NG):
            c, h = divmod(g, 8)
            if h == 0 and c + 1 < 32:
                for i1 in range(4 * (c + 1), 4 * (c + 1) + 4):
                    precast_load(C, pcb, i1)
            if g + 1 < NG:
                sc_mm(g + 1)
            post(g)
            ov_mm(g)
            if h == 7:
                OT = psb[4 + 2 * (c % 2)]; DEN = psb[5 + 2 * (c % 2)]
                P.recip(rden[:], DEN[0:64, :])
                o_ = oa[c % 2]
                P.tt(o_[:].rearrange("p h q -> p (h q)"), OT[0:64, :], rden[:], ALU.mult)
                P.dma(S["oaT"][:, :, c * 64:(c + 1) * 64], o_[:], outs=[(S["oaT"], c)])
                for i1 in range(4 * c, 4 * c + 4):
                    precast_cast(C, pcb, i1)


def phase3(C):
    nc, P, I, S, K, psb = C.nc, C.P, C.I, C.S, C.K, C.psb
    ident, onesf, onesb = K["ident"], K["onesf"], K["onesb"]
    with ExitStack() as es:
        def sb(name, shape, dt=F32):
            return es.enter_context(nc.sbuf_tensor("p3_" + name, list(shape), dt))

        wkv = sb("wkv", [128, 8, D], BF16)
        stg = [sb(f"stg{i}", [128, D]) for i in range(2)]
        for kc in range(8):
            P.dma(stg[kc % 2][:], I["w_mem_kv"][kc * 128:(kc + 1) * 128, :])
            P.copy(wkv[:, kc, :], stg[kc % 2][:], eng=("act", "dve")[kc % 2], outs=[(wkv, kc)])
        gm = sb("gm", [128, D]); mkn = sb("mkn", [128, 1])
        P.dma(gm[:], I["g_mem"].broadcast_to([128, D])); P.dma(mkn[:], I["mkn"])
        junk = sb("junk", [128, D]); ss = sb("ss", [128, 1]); rstd = sb("rstd", [128, 1])
        mb = sb("mb", [128, D], BF16)
        memT = sb("memT", [128, 8, 256], BF16)
        for ti in range(2):
            x_ = stg[ti]
            P.dma(x_[:], I["mem"][ti * 128:(ti + 1) * 128, :])
            P.act(junk[:], x_[:], AF.Square, accum_out=ss[:])
            P.act(ss[:], ss[:], AF.Sqrt, bias=EPS, scale=1.0 / D)
            P.recip(rstd[:], ss[:])
            P.stt(mb[:], x_[:], rstd[:], gm[:], ALU.mult, ALU.mult)
            pT = psb[0][:].bitcast(BF16)
            for kc in range(8):
                P.tr(pT[:, kc * 128:(kc + 1) * 128], mb[:, kc * 128:(kc + 1) * 128], ident[:], outs=[psb[0]])
            P.copy(memT[:, :, ti * 128:(ti + 1) * 128], pT[:, 0:1024].rearrange("p (k t) -> p k t", k=8),
                   eng="act", ins=[psb[0]])
        kmT = sb("kmT", [128, 4, 256], BF16)
        vm = sb("vm", [128, 2, 512], BF16)
        sq = sb("sq", [128, 256]); rs = sb("rs", [128, 256])
        for h in range(4):
            ps = psb[1]; ps2 = psb[2]
            for kc in range(8):
                P.mm(ps[:, 0:256], lhsT=wkv[:, kc, h * 128:(h + 1) * 128], rhs=memT[:, kc, :],
                     start=(kc == 0), stop=(kc == 7), outs=[ps])
            P.act(sq[:], ps[:, 0:256], AF.Square, ins=[ps])
            P.mm(ps2[:, 0:256], lhsT=onesf[:], rhs=sq[:], outs=[ps2])
            P.act(rs[:], ps2[:, 0:256], AF.Sqrt, bias=EPS, scale=1.0 / 128, ins=[ps2])
            P.recip(rs[:], rs[:])
            P.stt(kmT[:, h, :], ps[:, 0:256], mkn[:], rs[:], ALU.mult, ALU.mult, ins=[ps, mkn, rs],
                  outs=[(kmT, h)])
        for ti in range(2):
            ps = psb[3]
            for kc in range(8):
                P.mm(ps[:], lhsT=memT[:, kc, ti * 128:(ti + 1) * 128], rhs=wkv[:, kc, 512:1024],
                     start=(kc == 0), stop=(kc == 7))
            P.copy(vm[:, ti, :], ps[:], eng="act", outs=[(vm, ti)])
        qm = [sb(f"qm{i}", [128, 512], BF16) for i in range(2)]
        pT_ = [sb(f"pT{i}", [128, 2, 512], BF16) for i in range(2)]
        rden = sb("rden", [128, 512])
        om = [sb(f"om{i}", [128, 512], BF16) for i in range(2)]
        rdens = [rden, sb("rden2", [128, 512])]

        def sc3(n):
            b_, h = divmod(n, 4)
            q_ = qm[n % 2]
            P.dma(q_[:], S["qmT"][h, :, b_ * 512:(b_ + 1) * 512])
            for mt in range(2):
                ps = psb[mt + 6 * (n % 2)]
                P.mm(ps[:], lhsT=kmT[:, h, mt * 128:(mt + 1) * 128], rhs=q_[:])

        def ov3(n):
            b_, h = divmod(n, 4)
            p_ = pT_[n % 2]; o_ = om[n % 2]; rd = rdens[n % 2]
            for mt in range(2):
                ps = psb[mt + 6 * (n % 2)]
                P.act(p_[:, mt, :], ps[:], AF.Exp, outs=[(p_, mt)])
            OT = psb[2 + 2 * (n % 2)]; DEN = psb[3 + 2 * (n % 2)]
            for mt in range(2):
                P.mm(OT[:], lhsT=vm[:, mt, h * 128:(h + 1) * 128], rhs=p_[:, mt, :],
                     start=(mt == 0), stop=(mt == 1))
            for mt in range(2):
                P.mm(DEN[:], lhsT=onesb[:], rhs=p_[:, mt, :], start=(mt == 0), stop=(mt == 1))
            P.recip(rd[:], DEN[:])
            P.tt(o_[:], OT[:], rd[:], ALU.mult)
            P.dma(S["omT"][h, :, b_ * 512:(b_ + 1) * 512], o_[:], outs=[(S["omT"], (h, b_))])

        sc3(0)
        for n in range(16):
            if n + 1 < 16:
                sc3(n + 1)
            ov3(n)


def phase4(C):
    nc, P, I, S, K, psb = C.nc, C.P, C.I, C.S, C.K, C.psb
    ident, identf = K["ident"], K["identf"]
    with ExitStack() as es:
        def sb(name, shape, dt=F32):
            return es.enter_context(nc.sbuf_tensor("p4_" + name, list(shape), dt))

        iot = sb("iot", [128, 16, 16], BF16)
        P.op("pool", lambda e: e.iota(iot[:], pattern=[[0, 16], [1, 16]], base=0, channel_multiplier=0,
                                      allow_small_or_imprecise_dtypes=True), [], [iot])
        P.barrier()
        mg = sb("mg", [128, D]); h1 = sb("h1", [128, D])
        xt = sb("xt", [128, D])
        stg = [mg, h1, xt]
        wbrA = sb("wbrA", [64, 8, D], BF16)
        wbrB = sb("wbrB", [128, 4, D], BF16)
        wbrM = sb("wbrM", [128, 4, D], BF16)
        wo = sb("wo", [128, 8, D], BF16)
        wq = sb("wq", [128, 8, 2048], BF16)
        n = 0
        for h in range(8):
            st = stg[n % 3]
            P.dma(st[0:64, :], I["w_branch"][0, h * 64:(h + 1) * 64, :], outs=[st])
            P.copy(wbrA[:, h, :], st[0:64, :], eng=("act", "dve")[n % 2], ins=[st], outs=[(wbrA, h)])
            n += 1
        for bi, wt_ in ((1, wbrB), (2, wbrM)):
            for kc in range(4):
                st = stg[n % 3]
                P.dma(st[:], I["w_branch"][bi, kc * 128:(kc + 1) * 128, :])
                P.copy(wt_[:, kc, :], st[:], eng=("act", "dve")[n % 2], outs=[(wt_, kc)])
                n += 1
        for kc in range(8):
            st = stg[n % 3]
            P.dma(st[:], I["w_out"][kc * 128:(kc + 1) * 128, :])
            P.copy(wo[:, kc, :], st[:], eng=("act", "dve")[n % 2], outs=[(wo, kc)])
            n += 1
        for kc in range(8):
            for hf in range(2):
                st = stg[n % 3]
                P.dma(st[:], I["w_peer_q"][kc * 128:(kc + 1) * 128, hf * 1024:(hf + 1) * 1024])
                P.copy(wq[:, kc, hf * 1024:(hf + 1) * 1024], st[:], eng=("act", "dve")[n % 2],
                       outs=[(wq, (kc, hf))])
                n += 1
        wgt = sb("wgt", [128, 8, 3072], BF16)
        for kc in range(8):
            for g3 in range(3):
                st = stg[n % 3]
                P.dma(st[:], I["w_in"][kc * 128:(kc + 1) * 128, 4104 + g3 * 1024: 4104 + (g3 + 1) * 1024])
                P.copy(wgt[:, kc, g3 * 1024:(g3 + 1) * 1024], st[:], eng=("act", "dve")[n % 2],
                       outs=[(wgt, (kc, g3))])
                n += 1
        nTt = sb("nTt", [128, 8, 128], BF16)
        skT = sb("skT", [128, 2, 128])
        P.dma(skT[:], I["skT"])
        gf = sb("gf", [128, D])
        P.dma(gf[:], I["g_ffn"].broadcast_to([128, D]))

        oaT = sb("oaT", [64, 8, 128], BF16); obT = sb("obT", [128, 4, 128], BF16); omT = sb("omT", [128, 4, 128], BF16)
        gts = [sb(f"gts{i}", [128, 512]) for i in range(2)]
        mgb = sb("mgb", [128, D], BF16); mT = sb("mT", [128, 8, 128], BF16)
        ss = sb("ss", [128, 1]); rstd = sb("rstd", [128, 1])
        xnb = sb("xnb", [128, D], BF16); xnT = sb("xnT", [128, 8, 128], BF16)
        qTr = [sb(f"qT{i}", [128, 4, 128]) for i in range(2)]
        scr = [sb(f"sc{i}", [128, 16, 128]) for i in range(2)]
        sc2 = sb("sc2", [128, 128])
        v16 = sb("v16", [128, 16, 16]); ix = sb("ix", [128, 16, 16], U32); ixf = sb("ixf", [128, 16, 16])
        cand2 = sb("cand2", [128, 256])
        tv = sb("tv", [128, 8, 16]); pos = sb("pos", [128, 8, 16], U32)
        r1u = sb("r1u", [128, 8, 16], U32); r2u = sb("r2u", [128, 8, 16], U32)
        r1f = sb("r1f", [128, 8, 16], BF16); r2f = sb("r2f", [128, 8, 16], BF16)
        ixb = sb("ixb", [128, 16, 16], BF16)
        oh = sb("oh", [128, 8, 16, 16], BF16)
        i1f = sb("i1f", [128, 128]); i2f = sb("i2f", [128, 128])
        ge = sb("ge", [128, 8, 16]); gs = sb("gs", [128, 8]); gr = sb("gr", [128, 8]); gg = sb("gg", [128, 8, 16])
        trs = sb("trs", [128, 3, 128])
        pT = psb[2][:].bitcast(BF16)

        def partA(ti):
            sc = scr[ti % 2]
            ts_ = slice(ti * 128, (ti + 1) * 128)
            P.dma(oaT[:], S["oaT"][:, :, ts_])
            P.dma(obT[:], S["obT"][:, :, ts_].rearrange("k p t -> p k t"))
            P.dma(omT[:], S["omT"][:, :, ts_].rearrange("k p t -> p k t"))
            P.dma(nTt[:], S["nT"][:, :, ts_].rearrange("k p t -> p k t"))
            P.dma(xt[:], I["xo"][ts_, :])
            for br in range(3):
                for hf in range(2):
                    ps = psb[hf]
                    cs = slice(hf * 512, (hf + 1) * 512)
                    if br == 0:
                        for h in range(8):
                            P.mm(ps[:], lhsT=oaT[:, h, :], rhs=wbrA[:, h, cs], start=(h == 0), stop=(h == 7))
                    else:
                        src, w_ = (obT, wbrB) if br == 1 else (omT, wbrM)
                        for kc in range(4):
                            P.mm(ps[:], lhsT=src[:, kc, :], rhs=w_[:, kc, cs], start=(kc == 0), stop=(kc == 3))
                    gpsm = psb[4 + hf]
                    gco = br * 1024 + hf * 512
                    for kc in range(8):
                        P.mm(gpsm[:], lhsT=nTt[:, kc, :], rhs=wgt[:, kc, gco:gco + 512],
                             start=(kc == 0), stop=(kc == 7))
                    gts_ = gts[hf]
                    P.act(gts_[:], gpsm[:], AF.Sigmoid)
                    gsl = gts_[:]
                    if br == 0:
                        P.tt(mg[:, cs], ps[:], gsl, ALU.mult, ins=[ps, gts_], outs=[(mg, hf)])
                        yield
                    else:
                        P.tt(gts_[:], ps[:], gsl, ALU.mult, ins=[ps, gts_], outs=[gts_])
                        P.tt(mg[:, cs], mg[:, cs], gts_[:], ALU.add, eng="dve", ins=[(mg, hf), gts_],
                             outs=[(mg, hf)])
                    yield
            P.copy(mgb[:], mg[:], eng="act")
            yield
            for kc in range(8):
                P.tr(pT[:, kc * 128:(kc + 1) * 128], mgb[:, kc * 128:(kc + 1) * 128], ident[:], outs=[psb[2]])
            P.copy(mT[:], pT[:, 0:1024].rearrange("p (k t) -> p k t", k=8), eng="act", ins=[psb[2]])
            yield
            for hf in range(2):
                ps = psb[hf]
                cs = slice(hf * 512, (hf + 1) * 512)
                for kc in range(8):
                    P.mm(ps[:], lhsT=mT[:, kc, :], rhs=wo[:, kc, cs], start=(kc == 0), stop=(kc == 7))
                P.tt(h1[:, cs], ps[:], xt[:, cs], ALU.add, ins=[ps, xt], outs=[(h1, hf)])
                yield
            P.dma(S["h1"][ts_, :], h1[:], outs=[(S["h1"], ti)])
            P.act(xnb[:], h1[:], AF.Square, accum_out=ss[:])
            yield
            P.act(ss[:], ss[:], AF.Sqrt, bias=EPS, scale=1.0 / D)
            yield
            P.recip(rstd[:], ss[:])
            yield
            P.stt(xnb[:], h1[:], rstd[:], gf[:], ALU.mult, ALU.mult)
            yield
            for kc in range(8):
                P.tr(pT[:, kc * 128:(kc + 1) * 128], xnb[:, kc * 128:(kc + 1) * 128], ident[:], outs=[psb[2]])
            P.copy(xnT[:], pT[:, 0:1024].rearrange("p (k t) -> p k t", k=8), eng="act", ins=[psb[2]])
            yield
            P.dma(S["xn2T"][:, :, ts_].rearrange("k p t -> p k t"), xnT[:], outs=[(S["xn2T"], ti)])
            for g4 in range(4):
                ps = psb[2]
                qT = qTr[g4 % 2]
                for j in range(4):
                    ch = g4 * 4 + j
                    for kc in range(8):
                        P.mm(ps[:, j * 128:(j + 1) * 128], lhsT=wq[:, kc, ch * 128:(ch + 1) * 128],
                             rhs=xnT[:, kc, :], start=(kc == 0), stop=(kc == 7), outs=[ps])
                P.copy(qT[:], ps[:].rearrange("p (j t) -> p j t", j=4), eng="act", ins=[ps])
                yield
                ps2 = psb[4 + g4]
                for j in range(4):
                    ch = g4 * 4 + j
                    P.mm(ps2[:, j * 128:(j + 1) * 128], lhsT=qT[:, j, :], rhs=skT[:, ch % 2, :], outs=[ps2])
                P.copy(sc[:, g4 * 4:(g4 + 1) * 4, :], ps2[:].rearrange("p (j t) -> p j t", j=4),
                       eng="act", ins=[ps2], outs=[(sc, g4)])
                yield

        def partB(ti):
            sc = scr[ti % 2]
            ts_ = slice(ti * 128, (ti + 1) * 128)
            cand = sc[:].rearrange("p a b -> p (a b)").rearrange("p (h c) -> p h c", h=8)
            for hp in range(16):
                k_ = (sc, hp // 4)
                P.op("dve", lambda e, hp=hp: e.max(out=v16[:, hp, 0:8], in_=sc[:, hp, :]), [k_], [(v16, hp)])
                P.op("dve", lambda e, hp=hp: e.max_index(out=ix[:, hp, 0:8], in_max=v16[:, hp, 0:8],
                                                          in_values=sc[:, hp, :]), [k_, (v16, hp)], [(ix, hp)])
                P.op("dve", lambda e, hp=hp: e.match_replace(out=sc2[:], in_to_replace=v16[:, hp, 0:8],
                                                              in_values=sc[:, hp, :], imm_value=-1e30),
                     [k_, (v16, hp)], [sc2])
                yield
                P.op("dve", lambda e, hp=hp: e.max(out=v16[:, hp, 8:16], in_=sc2[:]), [sc2], [(v16, hp)])
                P.op("dve", lambda e, hp=hp: e.max_index(out=ix[:, hp, 8:16], in_max=v16[:, hp, 8:16],
                                                          in_values=sc2[:]), [sc2, (v16, hp)], [(ix, hp)])
                yield
            P.copy(ixf[:], ix[:])
            P.copy(ixb[:], ix[:], eng="act")
            v5 = v16[:].rearrange("p (h a) r -> p h a r", a=2)
            x5 = ixf[:].rearrange("p (h a) r -> p h a r", a=2)
            for h in range(8):
                P.tt(cand[:, h, :].rearrange("p (a b) -> p a b", a=16),
                     v5[:, h, 0, :].unsqueeze(2).to_broadcast([128, 16, 16]),
                     v5[:, h, 1, :].unsqueeze(1).to_broadcast([128, 16, 16]), ALU.add,
                     ins=[v16], outs=[sc])
                if h % 2 == 1:
                    yield
            for h in range(8):
                k_ = sc
                P.op("dve", lambda e, h=h: e.max(out=tv[:, h, 0:8], in_=cand[:, h, :]), [k_], [(tv, h)])
                P.op("dve", lambda e, h=h: e.max_index(out=pos[:, h, 0:8], in_max=tv[:, h, 0:8],
                                                        in_values=cand[:, h, :]), [k_, (tv, h)], [(pos, h)])
                P.op("dve", lambda e, h=h: e.match_replace(out=cand2[:], in_to_replace=tv[:, h, 0:8],
                                                            in_values=cand[:, h, :], imm_value=-1e30),
                     [k_, (tv, h)], [cand2])
                yield
                P.op("dve", lambda e, h=h: e.max(out=tv[:, h, 8:16], in_=cand2[:]), [cand2], [(tv, h)])
                P.op("dve", lambda e, h=h: e.max_index(out=pos[:, h, 8:16], in_max=tv[:, h, 8:16],
                                                        in_values=cand2[:]), [cand2, (tv, h)], [(pos, h)])
                yield
            P.ts(r1u[:], pos[:], 4, ALU.logical_shift_right)
            P.ts(r2u[:], pos[:], 15, ALU.bitwise_and)
            P.copy(r1f[:], r1u[:]); P.copy(r2f[:], r2u[:])
            xb5 = ixb[:].rearrange("p (h a) r -> p h a r", a=2)
            for a_, rf, dst in ((0, r1f, i1f), (1, r2f, i2f)):
                P.tt(oh[:], iot[:].unsqueeze(1).to_broadcast([128, 8, 16, 16]),
                     rf[:].unsqueeze(3).to_broadcast([128, 8, 16, 16]), ALU.is_equal)
                P.tt(oh[:], oh[:], xb5[:, :, a_, :].unsqueeze(2).to_broadcast([128, 8, 16, 16]), ALU.mult,
                     ins=[oh, ixb])
                P.reduce(dst[:], oh[:].rearrange("p h j r -> p (h j) r"))
                yield
            P.tt(ge[:], tv[:], tv[:, :, 0:1].to_broadcast([128, 8, 16]), ALU.subtract)
            P.act(ge[:], ge[:], AF.Exp)
            P.reduce(gs[:], ge[:])
            P.recip(gr[:], gs[:])
            P.tt(gg[:], ge[:], gr[:].unsqueeze(2).to_broadcast([128, 8, 16]), ALU.mult)
            yield
            tp = psb[3]
            P.tr(tp[:, 0:128], i1f[:], identf[:], outs=[tp])
            P.tr(tp[:, 128:256], i2f[:], identf[:], outs=[tp])
            P.tr(tp[:, 256:384], gg[:].rearrange("p h j -> p (h j)"), identf[:], outs=[tp])
            P.copy(trs[:], tp[:, 0:384].rearrange("p (a t) -> p a t", a=3), eng="act", ins=[tp])
            P.dma(S["i1T"][:, ts_], trs[:, 0, :], ins=[trs], outs=[(S["i1T"], ti)])
            P.dma(S["i2T"][:, ts_], trs[:, 1, :], ins=[trs], outs=[(S["i2T"], ti)])
            P.dma(S["gT"][:, ts_], trs[:, 2, :], ins=[trs], outs=[(S["gT"], ti)])
            yield

        def interleave(g1, g2, r1=1, r2=1):
            gens = [[g, r] for g, r in ((g1, r1), (g2, r2)) if g is not None]
            while gens:
                for gr_ in list(gens):
                    try:
                        for _ in range(gr_[1]):
                            next(gr_[0])
                    except StopIteration:
                        gens.remove(gr_)

        interleave(partA(0), None)
        for ti in range(16):
            interleave(partA(ti + 1) if ti + 1 < 16 else None, partB(ti), 1, 2)


TB = 256


def precast_load(C, bufs, i1):
    P, I = C.P, C.I
    ust, vst, ubs, vbs = bufs
    P.dma(ust[i1 % 8][:], I["utr"][i1])
    P.dma(vst[i1 % 8][:], I["peer_v"][i1 * 128:(i1 + 1) * 128, :])


def precast_cast(C, bufs, i1):
    P, S = C.P, C.S
    ust, vst, ubs, vbs = bufs
    ub_, vb_ = ubs[i1 % 4], vbs[i1 % 4]
    P.copy(ub_[:], ust[i1 % 8][:], eng="act")
    P.copy(vb_[:], vst[i1 % 8][:], eng="dve")
    P.dma(S["ub"][i1], ub_[:], outs=[(S["ub"], i1)])
    P.dma(S["vb"][i1 * 128:(i1 + 1) * 128, :], vb_[:], outs=[(S["vb"], i1)])


def phase5(C):
    nc, P, I, S, K, psb = C.nc, C.P, C.I, C.S, C.K, C.psb
    with ExitStack() as es:
        def sb(name, shape, dt=F32):
            return es.enter_context(nc.sbuf_tensor("p5_" + name, list(shape), dt))

        iob = sb("iob", [128, 128], BF16)
        P.op("pool", lambda e: e.iota(iob[:], pattern=[[1, 128]], base=0, channel_multiplier=0,
                                      allow_small_or_imprecise_dtypes=True), [], [iob])
        P.barrier()
        NB = T // TB
        WTs = [sb(f"WT{i}", [128, TB, 128], BF16) for i in range(2)]
        i1Ts = [sb(f"i1T{i}", [128, TB]) for i in range(2)]
        i2Ts = [sb(f"i2T{i}", [128, TB]) for i in range(2)]
        gTs = [sb(f"gT{i}", [128, TB]) for i in range(2)]
        xnTs = [sb(f"xnT{i}", [128, 8, TB], BF16) for i in range(2)]
        At = [sb(f"At{i}", [128, 128], BF16) for i in range(4)]
        Bt = [sb(f"Bt{i}", [128, 128], BF16) for i in range(4)]
        NR = 8
        ub = [sb(f"ub{i}", [128, 8, 128], BF16) for i in range(NR)]
        vb = [sb(f"vb{i}", [128, 1024], BF16) for i in range(NR)]
        ga = [sb(f"ga{i}", [128, TB], BF16) for i in range(2)]
        wg = [sb(f"wg{i}", [128, TB], BF16) for i in range(2)]
        h1 = sb("h1", [128, D]); ot = sb("ot", [128, D])

        def load_blk(blk):
            bs = slice(blk * TB, (blk + 1) * TB)
            k = blk % 2
            P.dma(i1Ts[k][:], S["i1T"][:, bs]); P.dma(i2Ts[k][:], S["i2T"][:, bs]); P.dma(gTs[k][:], S["gT"][:, bs])
            P.dma(xnTs[k][:], S["xn2T"][:, :, bs].rearrange("k p t -> p k t"))

        def wbuild(blk, t):
            k = blk % 2
            WT, i1T, i2T, gT = WTs[k], i1Ts[k], i2Ts[k], gTs[k]
            a_, b_ = At[t % 4], Bt[t % 4]
            P.ts(a_[:], iob[:], i1T[:, t:t + 1], ALU.is_equal, ins=[iob, i1T])
            P.ts(b_[:], iob[:], i2T[:, t:t + 1], ALU.is_equal, gT[:, t:t + 1], ALU.mult,
                 ins=[iob, i2T, gT])
            wp = psb[4]
            P.mm(wp[:, (t % 4) * 128:(t % 4 + 1) * 128], lhsT=b_[:], rhs=a_[:], outs=[wp])
            if t % 4 == 3:
                t0 = t - 3
                P.copy(WT[:, t0:t0 + 4, :],
                       wp[:].rearrange("p (t i) -> p t i", t=4), eng="act", ins=[wp],
                       outs=[(WT, t0)])

        load_blk(0)
        for t in range(TB):
            wbuild(0, t)
        OP = [[psb[0], psb[1]], [psb[2], psb[3]]]
        for blk in range(NB):
            k = blk % 2
            WT, xnT = WTs[k], xnTs[k]
            if blk + 1 < NB:
                load_blk(blk + 1)

            def ld(i1):
                ub_, vb_ = ub[i1 % NR], vb[i1 % NR]
                P.dma(ub_[:].rearrange("p k e -> p (k e)"), S["ub"][i1], ins=[(S["ub"], i1)])
                P.dma(vb_[:], S["vb"][i1 * 128:(i1 + 1) * 128, :], ins=[(S["vb"], i1)])

            def a_mm(i1):
                ub_ = ub[i1 % NR]
                ap_ = psb[5 + i1 % 3]
                for kc in range(8):
                    P.mm(ap_[:, 0:TB], lhsT=ub_[:, kc, :], rhs=xnT[:, kc, :], start=(kc == 0), stop=(kc == 7),
                         outs=[ap_])

            def o_mm(i1):
                vb_ = vb[i1 % NR]
                ap_ = psb[5 + i1 % 3]
                g_ = ga[i1 % 2]; w_ = wg[i1 % 2]
                P.act(g_[:], ap_[:, 0:TB], AF.Gelu, ins=[ap_])
                P.tt(w_[:], g_[:], WT[:, :, i1], ALU.mult, ins=[g_, WT])
                for sub in range(TB // 128):
                    for hf in range(2):
                        P.mm(OP[sub][hf][:], lhsT=w_[:, sub * 128:(sub + 1) * 128],
                             rhs=vb_[:, hf * 512:(hf + 1) * 512], start=(i1 == 0), stop=(i1 == 127))

            for j in range(NR - 1):
                ld(j)
            a_mm(0)
            a_mm(1)
            for i1 in range(128):
                if i1 + NR - 1 < 128:
                    ld(i1 + NR - 1)
                if i1 + 2 < 128:
                    a_mm(i1 + 2)
                o_mm(i1)
                if blk + 1 < NB:
                    for t in range(2 * i1, 2 * i1 + 2):
                        wbuild(blk + 1, t)
            for sub in range(TB // 128):
                r0 = blk * TB + sub * 128
                P.dma(h1[:], S["h1"][r0:r0 + 128, :])
                for hf in range(2):
                    P.tt(ot[:, hf * 512:(hf + 1) * 512], OP[sub][hf][:], h1[:, hf * 512:(hf + 1) * 512], ALU.add,
                         ins=[OP[sub][hf], h1], outs=[(ot, hf)])
                P.dma(C.out[r0:r0 + 128, :], ot[:], outs=[(C.out, r0)])


_NC = None


def _prep(inp):
    f = np.float32
    x = np.asarray(inp["x"], f)
    mem = np.asarray(inp["mem"], f)
    rb = np.asarray(inp["a_rel_bias"], f)[0]
    kk = np.arange(64)[:, None, None]
    ii = np.arange(9)[None, :, None]
    qq = np.arange(64)[None, None, :]
    rel = qq - ((ii - 8) * 64 + kk)
    idx = np.clip(rel, -128, 128) + 128
    abias = np.ascontiguousarray(rb[:, idx].transpose(1, 0, 2, 3).reshape(64, 8, 576))
    convw = np.ascontiguousarray(np.asarray(inp["b_conv"], f)[0, :, 0, :].reshape(4, 12, 128).transpose(2, 1, 0))
    pu = np.asarray(inp["peer_u"], f)[0]
    utr = np.ascontiguousarray(pu.reshape(128, 128, 8, 128).transpose(0, 3, 2, 1).reshape(128, 128, 1024))
    skT = np.ascontiguousarray(np.asarray(inp["peer_sub_keys"], f)[0].transpose(2, 0, 1))
    common = {
        "g_mix": np.asarray(inp["g_mix_norm"], f).reshape(1, D),
        "w_in": np.ascontiguousarray(np.asarray(inp["w_in"], f)[0]),
        "aqn": np.tile(np.asarray(inp["a_q_norm"], f)[0], 2).reshape(128, 1),
        "akn": np.tile(np.asarray(inp["a_k_norm"], f)[0], 2).reshape(128, 1),
        "abias": abias,
        "convw": convw,
        "b_a_log": np.asarray(inp["b_a_log"], f).reshape(1, 4),
        "b_dt_bias": np.asarray(inp["b_dt_bias"], f).reshape(1, 4),
        "b_out_norm": np.asarray(inp["b_out_norm"], f).reshape(1, 128),
        "g_mem": np.asarray(inp["g_mem_norm"], f).reshape(1, D),
        "w_mem_kv": np.ascontiguousarray(np.asarray(inp["w_mem_kv"], f)[0]),
        "mqn": np.asarray(inp["m_q_norm"], f).reshape(128, 1),
        "mkn": np.asarray(inp["m_k_norm"], f).reshape(128, 1),
        "w_branch": np.ascontiguousarray(np.asarray(inp["w_branch"], f)[0]),
        "w_out": np.ascontiguousarray(np.asarray(inp["w_out"], f)[0]),
        "g_ffn": np.asarray(inp["g_ffn_norm"], f).reshape(1, D),
        "w_peer_q": np.ascontiguousarray(np.asarray(inp["w_peer_q"], f)[0]),
        "skT": skT,
        "utr": utr,
        "peer_v": np.ascontiguousarray(np.asarray(inp["peer_v"], f)[0]),
    }
    hm0 = np.zeros((64, 8, 576), f)
    for c in range(8):
        hm0[:, c, :(8 - c) * 64] = NEG
    hm1 = np.zeros((64, 8, 576), f)
    zeros = np.zeros((T, D), f)
    maps = []
    for core in range(8):
        b, half = core // 2, core % 2
        m = dict(common)
        m["xo"] = np.ascontiguousarray(x[b, half * T:(half + 1) * T])
        m["xp"] = np.ascontiguousarray(x[b, 0:T]) if half == 1 else zeros
        m["mem"] = np.ascontiguousarray(mem[b])
        m["hmask"] = hm0 if half == 0 else hm1
        maps.append(m)
    return maps


def kernel(**inputs):
    global _NC
    if _NC is None:
        _NC = build_nc()
    maps = _prep(inputs)
    res = run_bass_kernel_spmd(_NC, maps, core_ids=list(range(8)))
    out = np.zeros((4, 2 * T, D), np.float32)
    for core in range(8):
        b, half = core // 2, core % 2
        out[b, half * T:(half + 1) * T] = np.asarray(res.results[core]["out"], np.float32)
    return out
```
